# Optimizing a Trainium2 kernel written in Bass

```python
import jax, jax.numpy as jnp
from jax import lax
import numpy as np

D_MODEL = 1024
BATCH = 8
SEQ = 2048
DEPTH = 2

N_MIXERS = 2
N_LAYERS_A = (DEPTH + 1) // 2
N_LAYERS_B = DEPTH // 2
PLE_DIM = 256
CONV_A_WIDTH = 3
D_RNN = D_MODEL
LRU_BLOCK = 256
N_LRU_BLOCKS = D_RNN // LRU_BLOCK
CONV_B_WIDTH = 4
RG_C = 8.0
N_GROUPS = 4
EXPERTS_PER_GROUP = 8
N_EXPERTS = N_GROUPS * EXPERTS_PER_GROUP
TOP_K = 2
D_EXPERT = 512
ROUTE_BLOCK = 128
EPS = 1e-6

kernel_name = 'hybrid_shortconv_rglru_hmoe_encoder'


def rmsnorm(x, g):
    x32 = x.astype(jnp.float32)
    y = x32 * lax.rsqrt(jnp.mean(x32 * x32, axis=-1, keepdims=True) + EPS)
    return (y * g.astype(jnp.float32)).astype(x.dtype)


def depthwise_conv(x, w, pad):
    return lax.conv_general_dilated(
        x, w[:, None, :], window_strides=(1,), padding=[pad],
        dimension_numbers=('NWC', 'WIO', 'NWC'), feature_group_count=x.shape[-1])


def short_conv_mixer(xn, w_in, conv_w, w_out):
    bgate, cgate, h = jnp.split(xn @ w_in, 3, axis=-1)
    u = depthwise_conv(cgate * h, conv_w, (1, 1))
    return (bgate * u) @ w_out


def _linear_combine(earlier, later):
    a_e, b_e = earlier
    a_l, b_l = later
    return a_l * a_e, a_l * b_e + b_l


def rglru_direction(uh, w_r, b_r, w_i, b_i, lam, reverse):
    bsz, s, nh, dh = uh.shape
    u = uh.reshape(bsz, s, nh * dh)
    r = jax.nn.sigmoid((jnp.einsum('bshd,hde->bshe', uh, w_r).reshape(bsz, s, -1) + b_r).astype(jnp.float32))
    i = jax.nn.sigmoid((jnp.einsum('bshd,hde->bshe', uh, w_i).reshape(bsz, s, -1) + b_i).astype(jnp.float32))
    log_a = -RG_C * r * jax.nn.softplus(-lam.astype(jnp.float32))
    a = jnp.exp(log_a)
    gated = jnp.sqrt(-jnp.expm1(2.0 * log_a)) * (i * u.astype(jnp.float32))
    _, h = lax.associative_scan(_linear_combine, (a, gated), axis=1, reverse=reverse)
    return h.astype(uh.dtype)


def rglru_mixer(xn, w_in, conv_w, conv_bias, w_rg, b_rg, w_ig, b_ig, lam, w_out):
    y_branch, u = jnp.split(xn @ w_in, 2, axis=-1)
    y_branch = jax.nn.gelu(y_branch)
    u = depthwise_conv(u, conv_w, (2, 1)) + conv_bias
    bsz, s, _ = u.shape
    uh = u.reshape(bsz, s, N_LRU_BLOCKS, LRU_BLOCK)
    h = (rglru_direction(uh, w_rg[0], b_rg[0], w_ig[0], b_ig[0], lam[0], False)
         + rglru_direction(uh, w_rg[1], b_rg[1], w_ig[1], b_ig[1], lam[1], True))
    return (y_branch * h) @ w_out


def hier_moe(x, w_rg, b_rg, w_re, b_re, w_gate, w_up, w_down):
    bsz, s, d = x.shape
    n = bsz * s
    xf = x.reshape(n, d)
    pg = jax.nn.softmax((xf @ w_rg).astype(jnp.float32) + b_rg.astype(jnp.float32), axis=-1)
    pg_top, g_top = lax.top_k(pg, 1)
    le = ((xf @ w_re).astype(jnp.float32) + b_re.astype(jnp.float32)).reshape(n, N_GROUPS, EXPERTS_PER_GROUP)
    le_sel = jnp.take_along_axis(le, g_top[:, :, None], axis=1)[:, 0]
    pe = jax.nn.softmax(le_sel, axis=-1)
    top_p, top_i = lax.top_k(pe, TOP_K)
    top_p = top_p / jnp.sum(top_p, axis=-1, keepdims=True)
    expert = g_top * EXPERTS_PER_GROUP + top_i
    weight = pg_top * top_p

    n_slots = n * TOP_K
    e_flat = expert.reshape(-1)
    tok_flat = jnp.arange(n_slots, dtype=jnp.int32) // TOP_K
    w_flat = weight.reshape(-1)
    order = jnp.argsort(e_flat)
    e_sorted = e_flat[order]
    counts = jnp.bincount(e_flat, length=N_EXPERTS)
    start = jnp.cumsum(counts) - counts
    padded = ((counts + ROUTE_BLOCK - 1) // ROUTE_BLOCK) * ROUTE_BLOCK
    pend = jnp.cumsum(padded)
    pstart = pend - padded
    dest = pstart[e_sorted] + (jnp.arange(n_slots) - start[e_sorted])
    n_blocks = -(-n_slots // ROUTE_BLOCK) + N_EXPERTS
    slot_of_pos = jnp.full((n_blocks * ROUTE_BLOCK,), n_slots, jnp.int32).at[dest].set(order.astype(jnp.int32))
    tok_pad = jnp.concatenate([tok_flat, jnp.array([n], jnp.int32)])[slot_of_pos]
    w_pad = jnp.concatenate([w_flat, jnp.zeros((1,), jnp.float32)])[slot_of_pos]
    block_expert = jnp.clip(jnp.searchsorted(pend, jnp.arange(n_blocks) * ROUTE_BLOCK, side='right'), 0, N_EXPERTS - 1)
    x_pad = jnp.concatenate([xf, jnp.zeros((1, d), xf.dtype)], axis=0)

    def expert_block(args):
        idx, e = args
        xb = x_pad[idx]
        hb = jax.nn.silu(xb @ w_gate[e]) * (xb @ w_up[e])
        return hb @ w_down[e]

    y_blocks = lax.map(expert_block, (tok_pad.reshape(n_blocks, ROUTE_BLOCK), block_expert))
    contrib = y_blocks.reshape(-1, d) * w_pad[:, None].astype(x.dtype)
    y = jnp.zeros((n + 1, d), x.dtype).at[tok_pad].add(contrib)
    return y[:n].reshape(bsz, s, d)


def _normal(key, shape, fan_in):
    return jax.random.normal(key, shape, jnp.float32) * (fan_in ** -0.5)


def setup_inputs(seed: int = 0) -> dict:
    key = jax.random.key(seed)
    ks = jax.random.split(key, 28)
    f32 = jnp.float32
    D, R, H, Bk = D_MODEL, D_RNN, N_LRU_BLOCKS, LRU_BLOCK
    x = jax.random.normal(ks[0], (BATCH, SEQ, D), f32)
    p = jax.random.normal(ks[1], (DEPTH, BATCH, SEQ, PLE_DIM), f32)
    norm_mix = 1.0 + 0.05 * jax.random.normal(ks[2], (DEPTH, D), f32)
    w_in_a = _normal(ks[3], (N_LAYERS_A, D, 3 * D), D)
    conv_a = _normal(ks[4], (N_LAYERS_A, CONV_A_WIDTH, D), CONV_A_WIDTH)
    w_out_a = _normal(ks[5], (N_LAYERS_A, D, D), D)
    w_in_b = _normal(ks[6], (N_LAYERS_B, D, 2 * R), D)
    conv_b = _normal(ks[7], (N_LAYERS_B, CONV_B_WIDTH, R), CONV_B_WIDTH)
    conv_bias_b = 0.01 * jax.random.normal(ks[8], (N_LAYERS_B, R), f32)
    w_rgate_b = _normal(ks[9], (N_LAYERS_B, 2, H, Bk, Bk), Bk)
    b_rgate_b = 0.01 * jax.random.normal(ks[10], (N_LAYERS_B, 2, R), f32)
    w_igate_b = _normal(ks[11], (N_LAYERS_B, 2, H, Bk, Bk), Bk)
    b_igate_b = 0.01 * jax.random.normal(ks[12], (N_LAYERS_B, 2, R), f32)
    a0 = jax.random.uniform(ks[13], (N_LAYERS_B, 2, R), f32, minval=0.9, maxval=0.999)
    sig = a0 ** (1.0 / RG_C)
    lam_b = jnp.log(sig) - jnp.log1p(-sig)
    w_out_b = _normal(ks[14], (N_LAYERS_B, R, D), R)
    norm_ffn = 1.0 + 0.05 * jax.random.normal(ks[15], (DEPTH, D), f32)
    w_router_group = _normal(ks[16], (DEPTH, D, N_GROUPS), D)
    b_router_group = 0.01 * jax.random.normal(ks[17], (DEPTH, N_GROUPS), f32)
    w_router_expert = _normal(ks[18], (DEPTH, D, N_EXPERTS), D)
    b_router_expert = 0.01 * jax.random.normal(ks[19], (DEPTH, N_EXPERTS), f32)
    w_exp_gate = _normal(ks[20], (DEPTH, N_EXPERTS, D, D_EXPERT), D)
    w_exp_up = _normal(ks[21], (DEPTH, N_EXPERTS, D, D_EXPERT), D)
    w_exp_down = _normal(ks[22], (DEPTH, N_EXPERTS, D_EXPERT, D), D_EXPERT)
    norm_ple = 1.0 + 0.05 * jax.random.normal(ks[23], (DEPTH, D), f32)
    w_ple = _normal(ks[24], (DEPTH, PLE_DIM, D), PLE_DIM)
    w_ple_gate = _normal(ks[25], (DEPTH, D, D), D)
    b_ple_gate = 0.01 * jax.random.normal(ks[26], (DEPTH, D), f32)
    norm_final = 1.0 + 0.05 * jax.random.normal(ks[27], (D,), f32)
    return {'x': x, 'p': p, 'norm_mix': norm_mix,
            'w_in_a': w_in_a, 'conv_a': conv_a, 'w_out_a': w_out_a,
            'w_in_b': w_in_b, 'conv_b': conv_b, 'conv_bias_b': conv_bias_b,
            'w_rgate_b': w_rgate_b, 'b_rgate_b': b_rgate_b,
            'w_igate_b': w_igate_b, 'b_igate_b': b_igate_b,
            'lam_b': lam_b, 'w_out_b': w_out_b, 'norm_ffn': norm_ffn,
            'w_router_group': w_router_group, 'b_router_group': b_router_group,
            'w_router_expert': w_router_expert, 'b_router_expert': b_router_expert,
            'w_exp_gate': w_exp_gate, 'w_exp_up': w_exp_up, 'w_exp_down': w_exp_down,
            'norm_ple': norm_ple, 'w_ple': w_ple, 'w_ple_gate': w_ple_gate,
            'b_ple_gate': b_ple_gate, 'norm_final': norm_final}


def reference(x, p, norm_mix, w_in_a, conv_a, w_out_a, w_in_b, conv_b, conv_bias_b,
              w_rgate_b, b_rgate_b, w_igate_b, b_igate_b, lam_b, w_out_b, norm_ffn,
              w_router_group, b_router_group, w_router_expert, b_router_expert,
              w_exp_gate, w_exp_up, w_exp_down, norm_ple, w_ple, w_ple_gate,
              b_ple_gate, norm_final):
    h = x
    for i in range(DEPTH):
        xn = rmsnorm(h, norm_mix[i])
        j = i // N_MIXERS
        if i % N_MIXERS == 0:
            mix = short_conv_mixer(xn, w_in_a[j], conv_a[j], w_out_a[j])
        else:
            mix = rglru_mixer(xn, w_in_b[j], conv_b[j], conv_bias_b[j], w_rgate_b[j], b_rgate_b[j],
                              w_igate_b[j], b_igate_b[j], lam_b[j], w_out_b[j])
        h = h + mix
        h = h + hier_moe(rmsnorm(h, norm_ffn[i]), w_router_group[i], b_router_group[i],
                         w_router_expert[i], b_router_expert[i],
                         w_exp_gate[i], w_exp_up[i], w_exp_down[i])
        gate = jax.nn.sigmoid(rmsnorm(h, norm_ple[i]) @ w_ple_gate[i] + b_ple_gate[i])
        h = h + gate * (p[i] @ w_ple[i])
    return rmsnorm(h, norm_final)
```

```python
import numpy as np
from contextlib import ExitStack
import concourse.bass as bass
import concourse.mybir as mybir
from concourse.bass_utils import run_bass_kernel_spmd

F32 = mybir.dt.float32
BF16 = mybir.dt.bfloat16
I32 = mybir.dt.int32
AF = mybir.ActivationFunctionType
ALU = mybir.AluOpType
AX = mybir.AxisListType

D = 1024
S = 2048
NCH = 8
TG = 4
TW = 512
NT = 16
NE = 32
DEPTH = 2
EPS = 1e-6
BIG = 1.0e30
CAP = 384
NSLOT = NE * CAP

V_GAIN = 0
V_CONVA = 7
V_CONVB = 10
V_CBIAS = 14
V_BR = 15
V_BI = 17
V_LAM = 19
V_BPLE = 21
NVB = 23

SCR_BYTES = 104 * 1024


class Prog:
    ENG = ("pe", "act", "dve", "pool", "sp")

    def __init__(self, nc, es):
        self.nc = nc
        self.es = es
        self.stream = {e: [] for e in self.ENG}
        self.semh = {}
        self.cnt = {}
        for e in ("pe", "act", "dve", "pool"):
            self._mksem("e:" + e)
        self.known = {e: {} for e in self.ENG}
        self.lastw = {}
        self.readers = {}
        self.ninst = 0
        self.bg_sems = set()
        self.bg_keys = set()

    def require(self, eng, sem):
        s = "d:" + sem
        self._emit_waits(eng, {s: self.cnt[s]})

    def _mksem(self, name):
        if name not in self.semh:
            self.semh[name] = self.es.enter_context(self.nc.semaphore(name.replace(":", "_")))
            self.cnt[name] = 0

    def _deps(self, reads, writes):
        need = {}

        def add(ev):
            s, v = ev
            if v > need.get(s, 0):
                need[s] = v
        for k in reads:
            if k in self.lastw:
                add(self.lastw[k])
        for k in writes:
            if k in self.lastw:
                add(self.lastw[k])
            for s, v in self.readers.get(k, {}).items():
                add((s, v))
        return need

    def _emit_waits(self, eng, need):
        for s, v in need.items():
            if eng == "pe" and s == "e:pe":
                continue
            if self.known[eng].get(s, 0) >= v:
                continue
            self.known[eng][s] = v
            h = self.semh[s]
            self.stream[eng].append(lambda e, h=h, v=v: e.wait_ge(h, v))

    def _commit(self, ev, reads, writes):
        ws = set(writes)
        for k in writes:
            self.lastw[k] = ev
            self.readers[k] = {}
        for k in reads:
            if k in ws:
                continue
            d = self.readers.setdefault(k, {})
            if ev[1] > d.get(ev[0], 0):
                d[ev[0]] = ev[1]

    def op(self, eng, fn, reads=(), writes=()):
        self._emit_waits(eng, self._deps(reads, writes))
        s = "e:" + eng
        self.cnt[s] += 1
        h = self.semh[s]
        self.stream[eng].append(lambda e, fn=fn, h=h: fn(e).then_inc(h, 1))
        self._commit((s, self.cnt[s]), reads, writes)
        self.ninst += 1

    def mm(self, fns, reads=(), writes=()):
        self._emit_waits("pe", self._deps(reads, writes))
        s = "e:pe"
        for f in fns[:-1]:
            self.stream["pe"].append(lambda e, f=f: f(e))
        self.cnt[s] += 1
        h = self.semh[s]
        last = fns[-1]
        self.stream["pe"].append(lambda e, last=last, h=h: last(e).then_inc(h, 1))
        self._commit((s, self.cnt[s]), reads, writes)
        self.ninst += len(fns)

    def dma(self, eng, sem, pairs, reads=(), writes=()):
        s = "d:" + sem
        self._mksem(s)
        self._emit_waits(eng, self._deps(reads, writes))
        h = self.semh[s]
        for (o, i) in pairs:
            self.cnt[s] += 16
            self.stream[eng].append(lambda e, o=o, i=i, h=h: e.dma_start(out=o, in_=i).then_inc(h, 16))
        self._commit((s, self.cnt[s]), reads, writes)
        self.ninst += len(pairs)

    def dmaf(self, eng, sem, fns, reads=(), writes=()):
        s = "d:" + sem
        self._mksem(s)
        self._emit_waits(eng, self._deps(reads, writes))
        h = self.semh[s]
        for f in fns:
            self.cnt[s] += 16
            self.stream[eng].append(lambda e, f=f, h=h: f(e).then_inc(h, 16))
        self._commit((s, self.cnt[s]), reads, writes)
        self.ninst += len(fns)

    def barrier(self, keep=(), skip_sems=()):
        saved = {k: self.lastw[k] for k in list(keep) + list(self.bg_keys) if k in self.lastw}
        skip = set("d:" + x for x in list(skip_sems) + list(self.bg_sems))
        for eng in self.ENG:
            need = {s: v for s, v in self.cnt.items() if v > 0 and s != "e:" + eng and s not in skip}
            if eng != "sp" and eng != "pe" and self.cnt["e:" + eng] > 0:
                need["e:" + eng] = self.cnt["e:" + eng]
            self._emit_waits(eng, need)
        self.lastw = dict(saved)
        self.readers = {}

    def finish(self):
        need = {s: v for s, v in self.cnt.items() if v > 0}
        self._emit_waits("sp", need)

    def replay(self):
        nc = self.nc
        st = self.stream
        with nc.Block() as block:
            @block.tensor
            def _(e):
                for f in st["pe"]:
                    f(e)

            @block.scalar
            def _(e):
                for f in st["act"]:
                    f(e)

            @block.vector
            def _(e):
                for f in st["dve"]:
                    f(e)

            @block.gpsimd
            def _(e):
                for f in st["pool"]:
                    f(e)

            @block.sync
            def _(e):
                for f in st["sp"]:
                    f(e)


def MM(o, l, r, st, sp):
    return lambda e: e.matmul(o, l, r, start=st, stop=sp)


def build_program(stop_after=None, moe_experts=NE):
    nc = bass.Bass("TRN2", target_bir_lowering=False)

    def din(name, shape):
        return nc.dram_tensor(name, list(shape), F32, kind="ExternalInput").ap()

    xT = din("xT", [D, S])
    pT = din("pT", [DEPTH, 256, S])
    vecs_d = din("vecs", [128, NVB * 8])
    wr_d = din("wr", [128, DEPTH * 8 * 36])
    br_d = din("br", [DEPTH * 36])
    cst_d = din("cst", [128, 288])
    w_in_a = din("w_in_a", [D, 3 * D])
    w_out_a = din("w_out_a", [D, D])
    w_in_b = din("w_in_b", [D, 2 * D])
    wg_d = din("wgates", [128, 8192])
    w_out_b = din("w_out_b", [D, D])
    weg = din("w_exp_gate", [DEPTH, NE, D, 512])
    weu = din("w_exp_up", [DEPTH, NE, D, 512])
    wed = din("w_exp_down", [DEPTH, NE, 512, D])
    w_ple = din("w_ple", [DEPTH, 256, D])
    w_pg = din("w_ple_gate", [DEPTH, D, D])
    yT = nc.dram_tensor("yT", [D, S], F32, kind="ExternalOutput").ap()
    Xs = nc.dram_tensor("Xs_scr", [NSLOT, D], BF16, kind="Internal").ap()
    Ys = nc.dram_tensor("Ys_scr", [NSLOT + 128, D], F32, kind="Internal").ap()

    es = ExitStack()
    with es:
        def sb(name, shape, dt):
            return es.enter_context(nc.sbuf_tensor(name, shape, dt))

        h = sb("h", [128, NCH, S], F32)
        xn = sb("xn", [128, NCH, S], BF16)
        scr = sb("scr", [128, SCR_BYTES // 2], BF16)
        vecs = sb("vecs_sb", [128, NVB * 8], F32)
        wr = sb("wr_sb", [128, DEPTH, 8, 36], F32)
        brb = sb("br_sb", [128, DEPTH, 36], F32)
        cst = sb("cst_sb", [128, 288], F32)
        ident = cst[:, 0:128]
        tri = cst[:, 128:256]
        ebase = cst[:, 256:288]
        identb = sb("identb", [128, 128], BF16)
        pw1 = sb("pw1", [128, NT], F32)
        pw2 = sb("pw2", [128, NT], F32)
        desti = [sb("desti0", [128, NT], I32), sb("desti1", [128, NT], I32)]
        destg = [sb("destg0", [128, NT], I32), sb("destg1", [128, NT], I32)]
        ones = sb("ones_sb", [128, 128], F32)
        onesb = sb("onesb_sb", [128, 128], BF16)
        rstd_tok = sb("rstd_tok", [128, NT], F32)
        cneg = sb("cneg", [128, 16], F32)
        cneg2 = sb("cneg2", [128, 16], F32)
        carry = sb("carry", [128, 1], F32)
        hbias = sb("hbias", [128, 32], F32)
        wrs = sb("wrs_p", [128, 8, 36], F32)
        idxY = sb("idxY", [128, NE * (CAP // 128)], I32)
        chh = sb("chh", [128, 16], F32)
        psA = es.enter_context(nc.psum_tensor("psA", [128, 2048], F32))
        psB = es.enter_context(nc.psum_tensor("psB", [128, 2048], F32))
        ps = [psA[:, i * TW:(i + 1) * TW] for i in range(4)] + [psB[:, i * TW:(i + 1) * TW] for i in range(4)]

        P = Prog(nc, es)
        _breg = {}

        def carve(off, shape, dt):
            n = int(np.prod(shape[1:]))
            nb = n * (4 if dt == F32 else 2)
            assert off % 4 == 0 and off + nb <= SCR_BYTES, (off, nb)
            a = scr[:, off // 2: (off + nb) // 2]
            if dt == F32:
                a = a.bitcast(F32)
            if len(shape) == 3:
                a = a.rearrange("p (a b) -> p a b", b=shape[2])
            elif len(shape) == 4:
                a = a.rearrange("p (a b c) -> p a b c", b=shape[2], c=shape[3])
            if shape[0] != 128:
                a = a[0:shape[0]]
            return a, off + nb

        def vcol(blk, c):
            return vecs[:, blk * 8 + c: blk * 8 + c + 1]

        def hk(c):
            return [("h", c, t) for t in range(TG)]

        XNK = [("xn", c) for c in range(NCH)]

        def tsl(t):
            return slice(t * TW, (t + 1) * TW)

        for c in range(NCH):
            P.dma("sp", "ldh%d" % c, [(h[:, c, :], xT[c * 128:(c + 1) * 128, :])], writes=hk(c))
        P.dma("sp", "ldc", [(vecs[:], vecs_d[:, :]),
                            (wr[:].rearrange("p l c j -> p (l c j)"), wr_d[:, :]),
                            (brb[:].rearrange("p l j -> p (l j)"), br_d.partition_broadcast(128)),
                            (cst[:], cst_d[:, :])],
              writes=["consts"])
        P.op("dve", lambda e: e.memset(ones[:], 1.0), writes=["ones"])
        P.op("dve", lambda e: e.memset(onesb[:], 1.0), writes=["onesb"])
        P.op("dve", lambda e: e.tensor_copy(out=identb[:], in_=ident), reads=["consts"], writes=["identb"])
        zt, _ = carve(24 * 1024, [128, 4, D], BF16)
        ztf = zt.rearrange("p a d -> p (a d)").bitcast(F32).rearrange("p (a d) -> p a d", d=D)
        P.op("pool", lambda e: e.memset(zt, 0.0), writes=["zt"])
        P.dma("sp", "zx", [(Xs[i * 512:(i + 1) * 512, :].rearrange("(j p) d -> p j d", p=128), zt) for i in range(NSLOT // 512)],
              reads=["zt"], writes=["Xs"])
        P.dma("sp", "zy", [(Ys[i * 256:(i + 1) * 256, :].rearrange("(j p) d -> p j d", p=128), ztf) for i in range(NSLOT // 256)]
              + [(Ys[NSLOT:NSLOT + 128, :], ztf[:, 0, :])], reads=["zt"], writes=["Ys"])
        P.bg_sems |= {"zx", "zy"}
        P.op("act", lambda e: e.activation(out=cneg[:], in_=vecs[:, V_LAM * 8:V_LAM * 8 + 16], func=AF.Exp, scale=-1.0),
             reads=["consts"], writes=["cneg"])
        P.op("act", lambda e: e.activation(out=cneg[:], in_=cneg[:], func=AF.Ln, bias=1.0),
             reads=["cneg"], writes=["cneg"])
        P.op("dve", lambda e: e.tensor_scalar(out=cneg2[:], in0=cneg[:], scalar1=-16.0, scalar2=None, op0=ALU.mult),
             reads=["cneg"], writes=["cneg2"])
        P.op("dve", lambda e: e.tensor_scalar(out=cneg[:], in0=cneg[:], scalar1=-8.0, scalar2=None, op0=ALU.mult),
             reads=["cneg"], writes=["cneg"])
        P.op("dve", lambda e: e.tensor_scalar(out=hbias[:], in0=vecs[:, V_BR * 8:V_BR * 8 + 32], scalar1=0.5, scalar2=None, op0=ALU.mult),
             reads=["consts"], writes=["hbias"])
        P.op("dve", lambda e: e.tensor_scalar(out=chh[:], in0=cneg[:], scalar1=0.5, scalar2=None, op0=ALU.mult),
             reads=["cneg"], writes=["chh"])

        def rmsnorm(idx, mode="bf", tok=False, hook=None):
            off = SCR_BYTES - 24 * 1024
            sq0, off = carve(off, [128, S], F32)
            sq1, off = carve(off, [128, S], F32)
            rstd, off = carve(off, [128, S], F32)
            sq = [sq0, sq1]
            sqb = [sq0.bitcast(BF16)[:, 0:S], sq0.bitcast(BF16)[:, S:2 * S], sq1.bitcast(BF16)[:, 0:S], sq1.bitcast(BF16)[:, S:2 * S]]
            for c in range(NCH):
                b = c % 4
                if c not in (2, 5):
                    P.op("act", lambda e, c=c, b=b: e.activation(out=sqb[b], in_=h[:, c, :], func=AF.Square),
                         reads=hk(c), writes=[("sqb", b)])
                else:
                    P.op("pool", lambda e, c=c, b=b: e.tensor_tensor(out=sqb[b], in0=h[:, c, :], in1=h[:, c, :], op=ALU.mult),
                         reads=hk(c), writes=[("sqb", b)])
                P.mm([MM(ps[t], onesb[:], sqb[b][:, tsl(t)], c == 0, c == NCH - 1) for t in range(TG)],
                     reads=[("sqb", b), "onesb"], writes=[("ps", t) for t in range(TG)])
            for t in range(TG):
                P.op("act", lambda e, t=t: e.activation(out=rstd[:, tsl(t)], in_=ps[t], func=AF.Sqrt,
                                                        scale=1.0 / D, bias=EPS),
                     reads=[("ps", t)], writes=[("rstd", t)])
                P.op("dve", lambda e, t=t: e.reciprocal(out=rstd[:, tsl(t)], in_=rstd[:, tsl(t)]),
                     reads=[("rstd", t)], writes=[("rstd", t)])
            if hook is not None:
                hook()
            RK = [("rstd", t) for t in range(TG)]
            for c in range(NCH):
                if mode == "bf":
                    P.op("dve", lambda e, c=c: e.scalar_tensor_tensor(
                        out=xn[:, c, :], in0=h[:, c, :], scalar=vcol(V_GAIN + idx, c), in1=rstd,
                        op0=ALU.mult, op1=ALU.mult), reads=hk(c) + RK + ["consts"], writes=[("xn", c)])
                else:
                    b = c % 2
                    P.op("dve", lambda e, c=c, b=b: e.scalar_tensor_tensor(
                        out=sq[b], in0=h[:, c, :], scalar=vcol(V_GAIN + idx, c), in1=rstd,
                        op0=ALU.mult, op1=ALU.mult), reads=hk(c) + RK + ["consts"], writes=[("sq", b)])
                    P.dma("sp", "st%d" % b, [(yT[c * 128:(c + 1) * 128, :], sq[b])], reads=[("sq", b)])
            if tok:
                P.mm([MM(ps[4][:, t:t + 1], rstd[:, t * 128:(t + 1) * 128], ident[:, 0:1], True, True)
                      for t in range(NT)], reads=RK + ["consts"], writes=[("ps", 4)])
                P.op("dve", lambda e: e.tensor_copy(out=rstd_tok[:], in_=ps[4][:, 0:NT]),
                     reads=[("ps", 4)], writes=["rstd_tok"])
            P.barrier()

        def dump_h():
            P.dma("sp", "st0", [(yT[c * 128:(c + 1) * 128, :], h[:, c, :]) for c in range(NCH)],
                  reads=[k for c in range(NCH) for k in hk(c)])
            P.finish()

        def mixer_a(pre=False):
            off = 0
            v, off = carve(off, [128, NCH, S], BF16)
            wout, off = carve(off, [128, NCH, D], BF16)
            win0, off = carve(off, [128, 3, NCH, 128], BF16)
            win1, off = carve(off, [128, 3, NCH, 128], BF16)
            csb, off = carve(off, [128, S], F32)
            ch, off = carve(off, [128, S], F32)
            u, off = carve(off, [128, S], F32)
            win = [win0, win1]
            def load_win(c):
                b = c % 2
                pairs = [(win[b][:, part, :, :],
                          w_in_a[:, part * D + c * 128: part * D + (c + 1) * 128].rearrange("(kc p) n -> p kc n", p=128))
                         for part in range(3)]
                P.dma("pool", "wAi%d" % b, pairs, writes=[("win", b)])
            if pre:
                load_win(0)
                load_win(1)
                P.dma("pool", "wAo", [(wout, w_out_a.rearrange("(kc p) d -> p kc d", p=128))], writes=["wout"])
                return
            for c in range(NCH):
                b = c % 2
                if c >= 2:
                    load_win(c)
                if c == 6:
                    P.require("dve", "zx")
                    P.require("dve", "zy")
                for (part, base) in ((1, 0), (2, 4)):
                    for t in range(TG):
                        P.mm([MM(ps[base + t], win[b][:, part, kc, :], xn[:, kc, tsl(t)], kc == 0, kc == NCH - 1)
                              for kc in range(NCH)], reads=[("win", b)] + XNK, writes=[("ps", base + t)])
                for t in range(TG):
                    P.op("act", lambda e, t=t: e.activation(out=csb[:, tsl(t)], in_=ps[t], func=AF.Copy),
                         reads=[("ps", t)], writes=[("csb", t)])
                for t in range(TG):
                    P.op("dve", lambda e, t=t: e.tensor_tensor(out=ch[:, tsl(t)], in0=csb[:, tsl(t)], in1=ps[4 + t], op=ALU.mult),
                         reads=[("csb", t), ("ps", 4 + t)], writes=[("ch", t)])
                CHK = [("ch", t) for t in range(TG)]
                P.op("dve", lambda e, c=c: e.tensor_scalar(out=u, in0=ch, scalar1=vcol(V_CONVA + 1, c), scalar2=None, op0=ALU.mult),
                     reads=CHK + ["consts"], writes=["u"])
                P.op("dve", lambda e, c=c: e.scalar_tensor_tensor(out=u[:, 1:S], in0=ch[:, 0:S - 1], scalar=vcol(V_CONVA + 0, c),
                                                                  in1=u[:, 1:S], op0=ALU.mult, op1=ALU.add),
                     reads=CHK + ["u"], writes=["u"])
                P.op("dve", lambda e, c=c: e.scalar_tensor_tensor(out=u[:, 0:S - 1], in0=ch[:, 1:S], scalar=vcol(V_CONVA + 2, c),
                                                                  in1=u[:, 0:S - 1], op0=ALU.mult, op1=ALU.add),
                     reads=CHK + ["u"], writes=["u"])
                for t in range(TG):
                    P.mm([MM(ps[t], win[b][:, 0, kc, :], xn[:, kc, tsl(t)], kc == 0, kc == NCH - 1)
                          for kc in range(NCH)], reads=[("win", b)] + XNK, writes=[("ps", t)])
                for t in range(TG):
                    P.op("dve", lambda e, c=c, t=t: e.tensor_tensor(out=v[:, c, tsl(t)], in0=ps[t], in1=u[:, tsl(t)], op=ALU.mult),
                         reads=[("ps", t), "u"], writes=[("v", c, t)])
            P.bg_sems -= {"zx", "zy"}
            out_proj(v, wout)

        def out_proj(v, wout):
            i = 0
            for dc in range(NCH):
                for t in range(TG):
                    bank = i % 8
                    i += 1
                    P.mm([MM(ps[bank], wout[:, kc, dc * 128:(dc + 1) * 128], v[:, kc, tsl(t)], kc == 0, kc == NCH - 1)
                          for kc in range(NCH)],
                         reads=["wout"] + [("v", kc, t) for kc in range(NCH)], writes=[("ps", bank)])
                    P.op("dve", lambda e, dc=dc, t=t, bank=bank: e.tensor_tensor(
                        out=h[:, dc, tsl(t)], in0=h[:, dc, tsl(t)], in1=ps[bank], op=ALU.add),
                        reads=[("ps", bank), ("h", dc, t)], writes=[("h", dc, t)])
            P.barrier()

        def mixer_b(pre=False):
            off = 0
            v, off = carve(off, [128, NCH, S], BF16)
            wgo, off = carve(off, [128, 8192], BF16)
            win0, off = carve(off, [128, 2, NCH, 128], BF16)
            win1, off = carve(off, [128, 2, NCH, 128], BF16)
            u32a, off = carve(off, [128, S], F32)
            u32b, off = carve(off, [128, S], F32)
            ubfa, off = carve(off, [128, S], BF16)
            ubfb, off = carve(off, [128, S], BF16)
            hacc, off = carve(off, [128, S], F32)
            HW = 1024
            rr, off = carve(off, [128, HW], F32)
            ii, off = carve(off, [128, HW], F32)
            aa, off = carve(off, [128, HW], F32)
            a2, off = carve(off, [128, HW], F32)
            win = [win0, win1]
            u32 = [u32a, u32b]
            ubf = [ubfa, ubfb]
            wg = wgo.rearrange("p (g d h k n) -> p g d h k n", g=2, d=2, h=4, k=2)
            tile_ctr = [0]
            def load_win(c):
                b = c % 2
                pairs = [(win[b][:, part, :, :],
                          w_in_b[:, part * D + c * 128: part * D + (c + 1) * 128].rearrange("(kc p) n -> p kc n", p=128))
                         for part in range(2)]
                P.dma("pool", "wBi%d" % b, pairs, writes=[("win", b)])
            if pre:
                load_win(0)
                load_win(1)
                P.dma("pool", "wBg", [(wgo.rearrange("p (a b) -> p a b", b=2048), wg_d.rearrange("p (a b) -> p a b", b=2048))],
                      writes=["wgo"])
                return
            for hd in range(4):
                for j in range(2):
                    c = 2 * hd + j
                    b = c % 2
                    if c >= 2:
                        load_win(c)
                    for t in range(TG):
                        P.mm([MM(ps[t], win[b][:, 1, kc, :], xn[:, kc, tsl(t)], kc == 0, kc == NCH - 1)
                              for kc in range(NCH)], reads=[("win", b)] + XNK, writes=[("ps", t)])
                    PSA = [("ps", t) for t in range(TG)]
                    uk = ("u32", j)
                    P.op("dve", lambda e, c=c, j=j: e.tensor_scalar(out=u32[j], in0=psA[:, :], scalar1=vcol(V_CONVB + 2, c),
                                                                    scalar2=vcol(V_CBIAS, c), op0=ALU.mult, op1=ALU.add),
                         reads=PSA + ["consts"], writes=[uk])
                    for (k, lo_o, lo_i, n) in ((0, 2, 0, S - 2), (1, 1, 0, S - 1), (3, 0, 1, S - 1)):
                        P.op("dve", lambda e, c=c, j=j, k=k, lo_o=lo_o, lo_i=lo_i, n=n: e.scalar_tensor_tensor(
                            out=u32[j][:, lo_o:lo_o + n], in0=psA[:, lo_i:lo_i + n], scalar=vcol(V_CONVB + k, c),
                            in1=u32[j][:, lo_o:lo_o + n], op0=ALU.mult, op1=ALU.add),
                            reads=PSA + [uk], writes=[uk])
                    P.op("act", lambda e, j=j: e.activation(out=ubf[j], in_=u32[j], func=AF.Copy),
                         reads=[uk], writes=[("ubf", j)])
                    for t in range(TG):
                        P.mm([MM(ps[4 + t], win[b][:, 0, kc, :], xn[:, kc, tsl(t)], kc == 0, kc == NCH - 1)
                              for kc in range(NCH)], reads=[("win", b)] + XNK, writes=[("ps", 4 + t)])
                        P.op("act", lambda e, c=c, t=t: e.activation(out=v[:, c, tsl(t)], in_=ps[4 + t], func=AF.Gelu_apprx_tanh),
                             reads=[("ps", 4 + t)], writes=[("v", c, t)])
                for j in range(2):
                    c = 2 * hd + j
                    for dr in range(2):
                        halves = (0, 1) if dr == 0 else (1, 0)
                        for hi, hf in enumerate(halves):
                            hs = slice(hf * HW, (hf + 1) * HW)
                            pb = 4 if (tile_ctr[0] % 2 == 0) else 0
                            tile_ctr[0] += 1
                            psG = psB if pb == 4 else psA
                            for (g, base) in ((0, pb), (1, pb + 2)):
                                for tt in range(2):
                                    t = hf * 2 + tt
                                    P.mm([MM(ps[base + tt], wg[:, g, dr, hd, k, j * 128:(j + 1) * 128], ubf[k][:, tsl(t)], k == 0, k == 1)
                                          for k in range(2)], reads=["wgo", ("ubf", 0), ("ubf", 1)], writes=[("ps", base + tt)])
                            col = dr * 8 + c
                            P.op("act", lambda e, col=col, psG=psG: e.activation(out=rr, in_=psG[:, 0:HW], func=AF.Tanh, scale=0.5,
                                                                                 bias=hbias[:, col:col + 1]),
                                 reads=[("ps", pb), ("ps", pb + 1), "hbias"], writes=["rr"])
                            P.op("act", lambda e, col=col, psG=psG: e.activation(out=ii, in_=psG[:, HW:2 * HW], func=AF.Tanh, scale=0.5,
                                                                                 bias=hbias[:, 16 + col:16 + col + 1]),
                                 reads=[("ps", pb + 2), ("ps", pb + 3), "hbias"], writes=["ii"])
                            P.op("act", lambda e, col=col: e.activation(out=aa, in_=rr, func=AF.Exp, scale=chh[:, col:col + 1],
                                                                        bias=chh[:, col:col + 1]),
                                 reads=["rr", "chh"], writes=["aa"])
                            P.op("act", lambda e, col=col: e.activation(out=a2, in_=rr, func=AF.Exp, scale=cneg[:, col:col + 1],
                                                                        bias=cneg[:, col:col + 1]),
                                 reads=["rr", "cneg"], writes=["a2"])
                            P.op("act", lambda e: e.activation(out=a2, in_=a2, func=AF.Sqrt, scale=-1.0, bias=1.0),
                                 reads=["a2"], writes=["a2"])
                            P.op("dve", lambda e, j=j, hs=hs: e.scalar_tensor_tensor(out=ii, in0=ii, scalar=1.0, in1=u32[j][:, hs],
                                                                                     op0=ALU.add, op1=ALU.mult),
                                 reads=["ii", ("u32", j)], writes=["ii"])
                            P.op("dve", lambda e: e.scalar_tensor_tensor(out=ii, in0=ii, scalar=0.5, in1=a2, op0=ALU.mult, op1=ALU.mult),
                                 reads=["ii", "a2"], writes=["ii"])
                            if dr == 0:
                                if hi == 0:
                                    init = 0.0
                                    rd = []
                                else:
                                    init = hacc[:, HW - 1:HW]
                                    rd = [("hacc", 0)]
                                P.op("dve", lambda e, hs=hs, init=init: e.tensor_tensor_scan(
                                    out=hacc[:, hs], data0=aa, data1=ii, initial=init, op0=ALU.mult, op1=ALU.add),
                                    reads=["aa", "ii"] + rd, writes=[("hacc", hf)])
                            else:
                                if hi == 0:
                                    init = 0.0
                                    rd = []
                                else:
                                    init = carry[:, 0:1]
                                    rd = ["carry"]
                                P.op("dve", lambda e, init=init: e.tensor_tensor_scan(
                                    out=a2[:, ::-1], data0=aa[:, ::-1], data1=ii[:, ::-1], initial=init,
                                    op0=ALU.mult, op1=ALU.add), reads=["aa", "ii", "a2"] + rd, writes=["a2"])
                                if hi == 0:
                                    P.op("dve", lambda e: e.tensor_copy(out=carry[:], in_=a2[:, 0:1]), reads=["a2"], writes=["carry"])
                                P.op("dve", lambda e, hs=hs: e.tensor_tensor(out=hacc[:, hs], in0=hacc[:, hs], in1=a2, op=ALU.add),
                                     reads=["a2", ("hacc", hf)], writes=[("hacc", hf)])
                    for t in range(TG):
                        P.op("dve", lambda e, c=c, t=t: e.tensor_tensor(out=v[:, c, tsl(t)], in0=v[:, c, tsl(t)], in1=hacc[:, tsl(t)], op=ALU.mult),
                             reads=[("v", c, t), ("hacc", t // 2)], writes=[("v", c, t)])
            wout = wgo.rearrange("p (kc d) -> p kc d", d=D)
            P.dma("pool", "wBg", [(wout, w_out_b.rearrange("(kc p) d -> p kc d", p=128))], writes=["wgo", "wout"])
            out_proj(v, wout)

        def moe(l):
            off = 0
            ar0, off = carve(off, [128, 12288], BF16)
            ar1, off = carve(off, [128, 12288], BF16)
            Hs0, off = carve(off, [128, 4, TW], BF16)
            Hs1, off = carve(off, [128, 4, TW], BF16)
            sg0, off = carve(off, [128, TW], F32)
            sg1, off = carve(off, [128, TW], F32)
            tt0, off = carve(off, [128, TW], F32)
            tt1, off = carve(off, [128, TW], F32)
            WT, off = carve(off, [128, S], F32)
            sel, off = carve(off, [128, NE, 128], F32)
            wrs, off = carve(off, [128, 8, 36], F32)
            L, off = carve(off, [128, NT, 36], F32)
            lm, off = carve(off, [128, NT, 32], F32)
            oh1, off = carve(off, [128, NT, 32], F32)
            oh2, off = carve(off, [128, NT, 32], F32)
            Wt, off = carve(off, [128, NT, 32], F32)
            g4a, off = carve(off, [128, NT, 4], F32)
            g4b, off = carve(off, [128, NT, 4], F32)
            s16 = []
            for _ in range(6):
                a, off = carve(off, [128, NT], F32)
                s16.append(a)
            gmax, gsum, m1, m2, pw1, pw2 = s16
            ar = [ar0, ar1]
            Hs = [Hs0, Hs1]
            sg = [sg0, sg1]
            ttm = [tt0, tt1]
            n_exp = moe_experts

            def load_expert(e_):
                b = e_ % 2
                a = ar[b]
                pairs = [(a[:, 0:4096].rearrange("p (kc f) -> p kc f", f=512), weg[l, e_].rearrange("(kc p) f -> p kc f", p=128)),
                         (a[:, 4096:8192].rearrange("p (kc f) -> p kc f", f=512), weu[l, e_].rearrange("(kc p) f -> p kc f", p=128)),
                         (a[:, 8192:12288].rearrange("p (fc d) -> p fc d", d=D), wed[l, e_].rearrange("(fc p) d -> p fc d", p=128))]
                P.dma("pool", "wE%d" % b, pairs, writes=[("ar", b)])

            load_expert(0)
            if n_exp > 1:
                load_expert(1)

            for c in range(NCH):
                P.op("dve", lambda e, c=c: e.tensor_scalar(out=wrs[:, c, :], in0=wr[:, l, c, :], scalar1=vcol(V_GAIN + 3 * l + 1, c),
                                                           scalar2=None, op0=ALU.mult), reads=["consts"], writes=["wrs"])
            P.op("dve", lambda e: e.tensor_copy(out=sel[0:32], in_=cst[0:32, 0:32].unsqueeze(2).to_broadcast([32, 32, 128])),
                 reads=["consts"], writes=["sel"])
            for t in range(NT):
                bank = t % 4
                P.mm([MM(ps[bank][:, 0:36], h[:, c, t * 128:(t + 1) * 128], wrs[:, c, :], c == 0, c == NCH - 1) for c in range(NCH)],
                     reads=["wrs"] + [("h", c, t // 4) for c in range(NCH)], writes=[("ps", bank)])
                P.op("dve", lambda e, t=t, bank=bank: e.scalar_tensor_tensor(
                    out=L[:, t, :], in0=ps[bank][:, 0:36], scalar=rstd_tok[:, t:t + 1], in1=brb[:, l, :],
                    op0=ALU.mult, op1=ALU.add), reads=[("ps", bank), "rstd_tok", "consts"], writes=["L"])
            gl = L[:, :, 0:4]
            le4 = L[:, :, 4:36].rearrange("p t (g e) -> p t g e", e=8)
            lm4 = lm.rearrange("p t (g e) -> p t g e", e=8)

            def bc3(a, n):
                return a.unsqueeze(2).to_broadcast([128, NT, n])
            R = "route"
            P.op("dve", lambda e: e.tensor_reduce(out=gmax, in_=gl, axis=AX.X, op=ALU.max), reads=["L"], writes=[R])
            P.op("dve", lambda e: e.tensor_tensor(out=g4a, in0=gl, in1=bc3(gmax, 4), op=ALU.is_equal), reads=["L", R], writes=[R])
            P.op("dve", lambda e: e.tensor_tensor(out=g4b, in0=gl, in1=bc3(gmax, 4), op=ALU.subtract), reads=["L", R], writes=[R])
            P.op("act", lambda e: e.activation(out=g4b, in_=g4b, func=AF.Exp), reads=[R], writes=[R])
            P.op("dve", lambda e: e.tensor_reduce(out=gsum, in_=g4b, axis=AX.X, op=ALU.add), reads=[R], writes=[R])
            P.op("dve", lambda e: e.reciprocal(out=gsum, in_=gsum), reads=[R], writes=[R])
            P.op("dve", lambda e: e.tensor_scalar(out=g4a, in0=g4a, scalar1=-1.0, scalar2=BIG, op0=ALU.add, op1=ALU.mult),
                 reads=[R], writes=[R])
            P.op("dve", lambda e: e.tensor_tensor(out=lm4, in0=le4, in1=g4a.unsqueeze(3).to_broadcast([128, NT, 4, 8]), op=ALU.add),
                 reads=["L", R], writes=[R])
            P.op("dve", lambda e: e.tensor_reduce(out=m1, in_=lm, axis=AX.X, op=ALU.max), reads=[R], writes=[R])
            P.op("dve", lambda e: e.tensor_tensor(out=oh1, in0=lm, in1=bc3(m1, 32), op=ALU.is_equal), reads=[R], writes=[R])
            P.op("dve", lambda e: e.scalar_tensor_tensor(out=lm, in0=oh1, scalar=-BIG, in1=lm, op0=ALU.mult, op1=ALU.add),
                 reads=[R], writes=[R])
            P.op("dve", lambda e: e.tensor_reduce(out=m2, in_=lm, axis=AX.X, op=ALU.max), reads=[R], writes=[R])
            P.op("dve", lambda e: e.tensor_tensor(out=oh2, in0=lm, in1=bc3(m2, 32), op=ALU.is_equal), reads=[R], writes=[R])
            P.op("dve", lambda e: e.tensor_tensor(out=m1, in0=m1, in1=m2, op=ALU.subtract), reads=[R], writes=[R])
            P.op("act", lambda e: e.activation(out=m1, in_=m1, func=AF.Sigmoid), reads=[R], writes=[R])
            P.op("dve", lambda e: e.tensor_tensor(out=pw1, in0=gsum, in1=m1, op=ALU.mult), reads=[R], writes=[R])
            P.op("dve", lambda e: e.tensor_tensor(out=pw2, in0=gsum, in1=pw1, op=ALU.subtract), reads=[R], writes=[R])
            P.op("dve", lambda e: e.tensor_tensor(out=Wt, in0=oh1, in1=bc3(pw1, 32), op=ALU.mult), reads=[R], writes=[R])
            P.op("dve", lambda e: e.tensor_tensor(out=oh2, in0=oh2, in1=bc3(pw2, 32), op=ALU.mult), reads=[R], writes=[R])
            P.op("dve", lambda e: e.tensor_tensor(out=Wt, in0=Wt, in1=oh2, op=ALU.add), reads=[R], writes=[R])
            for t4 in range(TG):
                P.mm([lambda e, t=t4 * 4 + q, q=q, t4=t4: e.transpose(ps[t4][0:32, q * 128:(q + 1) * 128], Wt[:, t, :], ident)
                      for q in range(4)], reads=[R, "consts"], writes=[("ps", t4)])
                P.op("act", lambda e, t4=t4: e.activation(out=WT[0:32, tsl(t4)], in_=ps[t4][0:32, :], func=AF.Copy),
                     reads=[("ps", t4)], writes=[("WT", t4)])

            def GU(step):
                e_, t = divmod(step, TG)
                b = e_ % 2
                a = ar[b]
                hb = step % 2
                wbk = 6 + hb
                P.mm([MM(ps[wbk], sel[0:32, e_, :], WT[0:32, tsl(t)], True, True)],
                     reads=["sel", ("WT", t)], writes=[("ps", wbk)])
                for fc in range(4):
                    q = fc % 2
                    P.mm([MM(ps[q], a[:, kc * 512 + fc * 128: kc * 512 + (fc + 1) * 128], xn[:, kc, tsl(t)], kc == 0, kc == NCH - 1)
                          for kc in range(NCH)], reads=[("ar", b)] + XNK, writes=[("ps", q)])
                    P.mm([MM(ps[2 + q], a[:, 4096 + kc * 512 + fc * 128: 4096 + kc * 512 + (fc + 1) * 128], xn[:, kc, tsl(t)], kc == 0, kc == NCH - 1)
                          for kc in range(NCH)], reads=[("ar", b)] + XNK, writes=[("ps", 2 + q)])
                    P.op("act", lambda e, q=q: e.activation(out=sg[q], in_=ps[q], func=AF.Silu),
                         reads=[("ps", q)], writes=[("sg", q)])
                    P.op("dve", lambda e, q=q: e.tensor_tensor(out=ttm[q], in0=sg[q], in1=ps[2 + q], op=ALU.mult),
                         reads=[("sg", q), ("ps", 2 + q)], writes=[("tt", q)])
                    P.op("dve", lambda e, q=q, hb=hb, fc=fc, wbk=wbk: e.tensor_tensor(out=Hs[hb][:, fc, :], in0=ttm[q], in1=ps[wbk], op=ALU.mult),
                         reads=[("tt", q), ("ps", wbk)], writes=[("Hs", hb, fc)])

            def YY(step):
                e_, t = divmod(step, TG)
                b = e_ % 2
                a = ar[b]
                hb = step % 2
                for dc in range(NCH):
                    yb = 4 + dc % 2
                    P.mm([MM(ps[yb], a[:, 8192 + fc * D + dc * 128: 8192 + fc * D + (dc + 1) * 128], Hs[hb][:, fc, :], fc == 0, fc == 3)
                          for fc in range(4)],
                         reads=[("ar", b)] + [("Hs", hb, fc) for fc in range(4)], writes=[("ps", yb)])
                    P.op("dve", lambda e, dc=dc, t=t, yb=yb: e.tensor_tensor(out=h[:, dc, tsl(t)], in0=h[:, dc, tsl(t)], in1=ps[yb], op=ALU.add),
                         reads=[("ps", yb), ("h", dc, t)], writes=[("h", dc, t)])

            nsteps = n_exp * TG
            for step in range(nsteps + 1):
                if step < nsteps:
                    GU(step)
                if step >= 1:
                    YY(step - 1)
                e_, t = divmod(step, TG)
                if step < nsteps and t == 0 and e_ >= 1 and e_ + 1 < n_exp:
                    load_expert(e_ + 1)
            P.barrier()

        def psR(t):
            return ps[t // 8][:, (t % 8) * 36:(t % 8) * 36 + 36]

        def router_prep(l):
            for c in range(NCH):
                P.op("dve", lambda e, c=c: e.tensor_scalar(out=wrs[:, c, :], in0=wr[:, l, c, :], scalar1=vcol(V_GAIN + 3 * l + 1, c),
                                                           scalar2=None, op0=ALU.mult), reads=["consts"], writes=["wrs"])

        def router_mm():
            off_ = SCR_BYTES - 24 * 1024
            LT, _ = carve(off_, [128, S], F32)
            for t4 in range(TG):
                P.mm([MM(ps[4 + t4][0:36, :], wrs[:, c, :], h[:, c, tsl(t4)], c == 0, c == NCH - 1) for c in range(NCH)],
                     reads=["wrs"] + [("h", c, t4) for c in range(NCH)], writes=[("ps", 4 + t4)])
            for t4 in range(TG):
                P.op("act", lambda e, t4=t4: e.activation(out=LT[0:36, tsl(t4)], in_=ps[4 + t4][0:36, :], func=AF.Copy),
                     reads=[("ps", 4 + t4)], writes=[("LT", t4)] + ([("sqb", 0), ("sqb", 1)] if t4 < 2 else []))
            for t in range(NT):
                P.mm([lambda e, t=t: e.transpose(psR(t), LT[0:36, t * 128:(t + 1) * 128], ident[0:36, 0:36])],
                     reads=[("LT", t // 4), "consts"], writes=[("ps", t // 8)])

        def moe_sorted(l):
            IOA = bass.IndirectOffsetOnAxis
            off = 0

            def breg(e):
                if 'r' not in _breg:
                    _breg['r'] = e.to_reg(NSLOT - 1)
                return _breg['r']

            def breg2(e):
                if 'r2' not in _breg:
                    _breg['r2'] = e.to_reg(NSLOT + 127)
                return _breg['r2']
            ar0, off = carve(off, [128, 12288], BF16)
            ar1, off = carve(off, [128, 12288], BF16)
            base = off
            ar = [ar0, ar1]

            def load_part(e_, part):
                b = e_ % 2
                a = ar[b]
                if part == 0:
                    pr = (a[:, 0:4096].rearrange("p (kc f) -> p kc f", f=512), weg[l, e_].rearrange("(kc p) f -> p kc f", p=128))
                elif part == 1:
                    pr = (a[:, 4096:8192].rearrange("p (kc f) -> p kc f", f=512), weu[l, e_].rearrange("(kc p) f -> p kc f", p=128))
                else:
                    pr = (a[:, 8192:12288].rearrange("p (fc d) -> p fc d", d=D), wed[l, e_].rearrange("(fc p) d -> p fc d", p=128))
                P.dma("pool", "wE%d%d" % (b, part), [pr], writes=[("ar", b, part)])

            for e0 in range(2):
                for part in range(3):
                    load_part(e0, part)
            ARK = [("ar", b_, p_) for b_ in range(2) for p_ in range(3)]
            ARS = ["wE%d%d" % (b_, p_) for b_ in range(2) for p_ in range(3)]

            off = base
            xt, off = carve(off, [128, NT, D], BF16)
            L, off = carve(off, [128, NT, 36], F32)
            lm, off = carve(off, [128, NT, 32], F32)
            oh1, off = carve(off, [128, NT, 32], F32)
            oh2, off = carve(off, [128, NT, 32], F32)
            Cc, off = carve(off, [128, NT, 32], F32)
            Ccum, off = carve(off, [128, NT, 32], F32)
            Rr, off = carve(off, [128, NT, 32], F32)
            g4a, off = carve(off, [128, NT, 4], F32)
            g4b, off = carve(off, [128, NT, 4], F32)
            s16 = []
            for _ in range(8):
                a_, off = carve(off, [128, NT], F32)
                s16.append(a_)
            gmax, gsum, m1, m2, dstf, rkf, ovf, _sp = s16

            for t in range(NT):
                P.op("dve", lambda e, t=t: e.scalar_tensor_tensor(
                    out=L[:, t, :], in0=psR(t), scalar=rstd_tok[:, t:t + 1], in1=brb[:, l, :],
                    op0=ALU.mult, op1=ALU.add), reads=[("ps", t // 8), "rstd_tok", "consts"], writes=["L"])
            for t in range(NT):
                bank = 4 + t % 4
                psb = ps[bank].bitcast(BF16)
                P.mm([lambda e, c=c, t=t, psb=psb: e.transpose(psb[:, c * 128:(c + 1) * 128], xn[:, c, t * 128:(t + 1) * 128], identb[:])
                      for c in range(NCH)], reads=XNK + ["identb"], writes=[("ps", bank)])
                if t % 2 == 0:
                    P.op("act", lambda e, t=t, psb=psb: e.activation(out=xt[:, t, :], in_=psb, func=AF.Copy),
                         reads=[("ps", bank)], writes=[("xt", t)])
                else:
                    P.op("dve", lambda e, t=t, psb=psb: e.tensor_copy(out=xt[:, t, :], in_=psb),
                         reads=[("ps", bank)], writes=[("xt", t)])
            gl = L[:, :, 0:4]
            le4 = L[:, :, 4:36].rearrange("p t (g e) -> p t g e", e=8)
            lm4 = lm.rearrange("p t (g e) -> p t g e", e=8)

            def bc3(a, n):
                return a.unsqueeze(2).to_broadcast([128, NT, n])
            R = "route"
            P.op("dve", lambda e: e.tensor_reduce(out=gmax, in_=gl, axis=AX.X, op=ALU.max), reads=["L"], writes=[R])
            P.op("dve", lambda e: e.tensor_tensor(out=g4a, in0=gl, in1=bc3(gmax, 4), op=ALU.is_equal), reads=["L", R], writes=[R])
            P.op("dve", lambda e: e.tensor_tensor(out=g4b, in0=gl, in1=bc3(gmax, 4), op=ALU.subtract), reads=["L", R], writes=[R])
            P.op("act", lambda e: e.activation(out=g4b, in_=g4b, func=AF.Exp), reads=[R], writes=[R])
            P.op("dve", lambda e: e.tensor_reduce(out=gsum, in_=g4b, axis=AX.X, op=ALU.add), reads=[R], writes=[R])
            P.op("dve", lambda e: e.reciprocal(out=gsum, in_=gsum), reads=[R], writes=[R])
            P.op("dve", lambda e: e.tensor_scalar(out=g4a, in0=g4a, scalar1=-1.0, scalar2=BIG, op0=ALU.add, op1=ALU.mult),
                 reads=[R], writes=[R])
            P.op("dve", lambda e: e.tensor_tensor(out=lm4, in0=le4, in1=g4a.unsqueeze(3).to_broadcast([128, NT, 4, 8]), op=ALU.add),
                 reads=["L", R], writes=[R])
            P.op("dve", lambda e: e.tensor_reduce(out=m1, in_=lm, axis=AX.X, op=ALU.max), reads=[R], writes=[R])
            P.op("dve", lambda e: e.tensor_tensor(out=oh1, in0=lm, in1=bc3(m1, 32), op=ALU.is_equal), reads=[R], writes=[R])
            P.op("dve", lambda e: e.scalar_tensor_tensor(out=lm, in0=oh1, scalar=-BIG, in1=lm, op0=ALU.mult, op1=ALU.add),
                 reads=[R], writes=[R])
            P.op("dve", lambda e: e.tensor_reduce(out=m2, in_=lm, axis=AX.X, op=ALU.max), reads=[R], writes=[R])
            P.op("dve", lambda e: e.tensor_tensor(out=oh2, in0=lm, in1=bc3(m2, 32), op=ALU.is_equal), reads=[R], writes=[R])
            P.op("dve", lambda e: e.tensor_tensor(out=m1, in0=m1, in1=m2, op=ALU.subtract), reads=[R], writes=[R])
            P.op("act", lambda e: e.activation(out=m1, in_=m1, func=AF.Sigmoid), reads=[R], writes=[R])
            P.op("dve", lambda e: e.tensor_tensor(out=pw1[:], in0=gsum, in1=m1, op=ALU.mult), reads=[R], writes=[R, "pw"])
            P.op("dve", lambda e: e.tensor_tensor(out=pw2[:], in0=gsum, in1=pw1[:], op=ALU.subtract), reads=[R], writes=[R, "pw"])
            P.op("dve", lambda e: e.tensor_tensor(out=Cc, in0=oh1, in1=oh2, op=ALU.add), reads=[R], writes=[R])
            P.op("dve", lambda e: e.memset(Ccum[:, 0, :], 0.0), reads=[R], writes=[R])
            for t in range(1, NT):
                P.op("dve", lambda e, t=t: e.tensor_tensor(out=Ccum[:, t, :], in0=Ccum[:, t - 1, :], in1=Cc[:, t - 1, :], op=ALU.add),
                     reads=[R], writes=[R])
            NB = NE * (CAP // 128)
            rowb, off = carve(off, [128, NB], F32)
            thr, off = carve(off, [128, NB], F32)
            vv, off = carve(off, [128, NB], F32)
            P.op("pool", lambda e: e.iota(rowb, pattern=[[128, NB]], base=0, channel_multiplier=1, allow_small_or_imprecise_dtypes=True),
                 writes=["rowb"])
            P.op("pool", lambda e: e.iota(thr, pattern=[[0, NE], [128, CAP // 128]], base=0, channel_multiplier=1,
                                          allow_small_or_imprecise_dtypes=True), writes=["thr"])
            P.op("dve", lambda e: e.tensor_tensor(out=lm[:, 0, :], in0=Ccum[:, NT - 1, :], in1=Cc[:, NT - 1, :], op=ALU.add),
                 reads=[R], writes=[R])
            P.mm([MM(ps[1][:, 0:NE], ones[:], lm[:, 0, :], True, True)], reads=[R, "ones"], writes=[("ps", 1)])
            P.op("dve", lambda e: e.tensor_tensor(out=vv.rearrange("p (e j) -> p e j", j=CAP // 128),
                                                  in0=thr.rearrange("p (e j) -> p e j", j=CAP // 128),
                                                  in1=ps[1][:, 0:NE].unsqueeze(2).to_broadcast([128, NE, CAP // 128]), op=ALU.is_lt),
                 reads=[("ps", 1), "thr"], writes=["vv"])
            P.op("dve", lambda e: e.tensor_scalar(out=vv, in0=vv, scalar1=-1.0, scalar2=-1.0e6, op0=ALU.add, op1=ALU.mult),
                 reads=["vv"], writes=["vv"])
            P.op("dve", lambda e: e.tensor_tensor(out=vv, in0=vv, in1=rowb, op=ALU.add), reads=["vv", "rowb"], writes=["vv"])
            P.op("dve", lambda e: e.tensor_copy(out=idxY[:], in_=vv), reads=["vv"], writes=["idxY"])
            Cf = Cc.rearrange("p t e -> p (t e)")
            Ccf = Ccum.rearrange("p t e -> p (t e)")
            P.mm([MM(ps[0], tri, Cf, True, False), MM(ps[0], ones[:], Ccf, False, True)],
                 reads=[R, "consts", "ones"], writes=[("ps", 0)])
            rk3 = ps[0].rearrange("p (t e) -> p t e", e=32)
            P.op("dve", lambda e: e.tensor_tensor(out=Rr, in0=rk3, in1=ebase.unsqueeze(1).to_broadcast([128, NT, 32]), op=ALU.add),
                 reads=[("ps", 0), "consts"], writes=[R])
            for k, oh in enumerate((oh1, oh2)):
                P.op("dve", lambda e, oh=oh: e.tensor_tensor(out=lm, in0=oh, in1=Rr, op=ALU.mult), reads=[R], writes=[R])
                P.op("dve", lambda e: e.tensor_reduce(out=dstf, in_=lm, axis=AX.X, op=ALU.add), reads=[R], writes=[R])
                P.op("dve", lambda e, oh=oh: e.tensor_tensor(out=lm, in0=oh, in1=rk3, op=ALU.mult), reads=[R, ("ps", 0)], writes=[R])
                P.op("dve", lambda e: e.tensor_reduce(out=rkf, in_=lm, axis=AX.X, op=ALU.add), reads=[R], writes=[R])
                P.op("dve", lambda e: e.tensor_scalar(out=ovf, in0=rkf, scalar1=float(CAP), scalar2=1.0e6, op0=ALU.is_ge, op1=ALU.mult),
                     reads=[R], writes=[R])
                P.op("dve", lambda e: e.tensor_tensor(out=dstf, in0=dstf, in1=ovf, op=ALU.add), reads=[R], writes=[R])
                P.op("dve", lambda e, k=k: e.tensor_copy(out=desti[k][:], in_=dstf), reads=[R], writes=[R, ("desti", k)])
                P.op("dve", lambda e: e.tensor_scalar(out=dstf, in0=dstf, scalar1=float(NSLOT), scalar2=None, op0=ALU.min), reads=[R], writes=[R])
                P.op("dve", lambda e, k=k: e.tensor_copy(out=destg[k][:], in_=dstf), reads=[R], writes=[R, ("destg", k)])
            fns = []
            for t in range(NT):
                for k in range(2):
                    fns.append(lambda e, t=t, k=k: e.indirect_dma_start(
                        out=Xs[:, :], out_offset=IOA(ap=desti[k][:, t:t + 1], axis=0), in_=xt[:, t, :], in_offset=None,
                        bounds_check=breg(e), oob_is_err=False))
            P.dmaf("pool", "sc", fns, reads=[("xt", t) for t in range(NT)] + [("desti", 0), ("desti", 1)], writes=["Xs"])
            P.barrier(keep=ARK, skip_sems=ARS)

            off = base
            Xb0, off = carve(off, [128, 3, D], BF16)
            Xb1, off = carve(off, [128, 3, D], BF16)
            XT0, off = carve(off, [128, NCH, CAP], BF16)
            XT1, off = carve(off, [128, NCH, CAP], BF16)
            Hs0, off = carve(off, [128, 4, CAP], BF16)
            Hs1, off = carve(off, [128, 4, CAP], BF16)
            sg0, off = carve(off, [128, CAP], F32)
            sg1, off = carve(off, [128, CAP], F32)
            Yb = []
            for _ in range(3):
                a_, off = carve(off, [128, D], F32)
                Yb.append(a_)
            Xb = [Xb0, Xb1]
            XT = [XT0, XT1]
            Hs = [Hs0, Hs1]
            sg = [sg0, sg1]
            NJ = CAP // 128
            def xb_load(e_):
                b = e_ % 2
                P.dmaf("pool", "xb%d" % b, [lambda e, j=j, b=b, e_=e_: e.indirect_dma_start(
                    out=Xb[b][:, j, :], out_offset=None, in_=Xs[:, :],
                    in_offset=IOA(ap=idxY[:, e_ * NJ + j:e_ * NJ + j + 1], axis=0), bounds_check=breg(e), oob_is_err=False)
                    for j in range(NJ)], reads=["Xs"], writes=[("Xb", b)])

            def do_transposes(e_):
                b = e_ % 2
                for j in range(NJ):
                    tb = 6 + (e_ * NJ + j) % 2
                    psb = ps[tb].bitcast(BF16)
                    P.mm([lambda e, c=c, j=j, b=b, psb=psb: e.transpose(psb[:, c * 128:(c + 1) * 128], Xb[b][:, j, c * 128:(c + 1) * 128], identb[:])
                          for c in range(NCH)], reads=[("Xb", b), "identb"], writes=[("ps", tb)])
                    P.op("act", lambda e, j=j, b=b, psb=psb: e.activation(
                        out=XT[b][:, :, j * 128:(j + 1) * 128], in_=psb.rearrange("p (c n) -> p c n", n=128), func=AF.Copy),
                        reads=[("ps", tb)], writes=[("XT", b, j)])

            for b_ in range(2):
                P.op("dve", lambda e, b_=b_: e.memset(Xb[b_], 0.0), writes=[("Xb", b_)])
            xb_load(0)
            xb_load(1)
            do_transposes(0)
            for e_ in range(NE):
                b = e_ % 2
                a = ar[b]
                XTK = [("XT", b, j) for j in range(NJ)]
                for fc in range(4):
                    q = fc % 2
                    P.mm([MM(ps[q][:, 0:CAP], a[:, kc * 512 + fc * 128: kc * 512 + (fc + 1) * 128], XT[b][:, kc, :], kc == 0, kc == NCH - 1)
                          for kc in range(NCH)], reads=[("ar", b, 0)] + XTK, writes=[("ps", q)])
                    P.mm([MM(ps[2 + q][:, 0:CAP], a[:, 4096 + kc * 512 + fc * 128: 4096 + kc * 512 + (fc + 1) * 128], XT[b][:, kc, :], kc == 0, kc == NCH - 1)
                          for kc in range(NCH)], reads=[("ar", b, 1)] + XTK, writes=[("ps", 2 + q)])
                    P.op("act", lambda e, q=q: e.activation(out=sg[q], in_=ps[q][:, 0:CAP], func=AF.Silu),
                         reads=[("ps", q)], writes=[("sg", q)])
                    P.op("dve", lambda e, q=q, b=b, fc=fc: e.tensor_tensor(out=Hs[b][:, fc, :], in0=sg[q], in1=ps[2 + q][:, 0:CAP], op=ALU.mult),
                         reads=[("sg", q), ("ps", 2 + q)], writes=[("Hs", b, fc)])
                if e_ + 2 < NE:
                    load_part(e_ + 2, 0)
                    load_part(e_ + 2, 1)
                if e_ + 1 < NE:
                    do_transposes(e_ + 1)
                if e_ + 2 < NE:
                    xb_load(e_ + 2)
                HK = [("Hs", b, fc) for fc in range(4)]
                for j in range(NJ):
                    yb = (e_ * NJ + j) % 3
                    for half in range(2):
                        bank = 4 + half
                        P.mm([MM(ps[bank], Hs[b][:, fc, j * 128:(j + 1) * 128], a[:, 8192 + fc * D + half * 512: 8192 + fc * D + (half + 1) * 512], fc == 0, fc == 3)
                              for fc in range(4)], reads=[("ar", b, 2)] + HK, writes=[("ps", bank)])
                        if half == 0:
                            P.op("act", lambda e, yb=yb, bank=bank: e.activation(out=Yb[yb][:, 0:512], in_=ps[bank], func=AF.Copy),
                                 reads=[("ps", bank)], writes=[("Yb", yb, 0)])
                        else:
                            P.op("dve", lambda e, yb=yb, bank=bank: e.tensor_copy(out=Yb[yb][:, 512:1024], in_=ps[bank]),
                                 reads=[("ps", bank)], writes=[("Yb", yb, 1)])
                    r0 = (e_ * NJ + j) * 128
                    P.dmaf("pool", "ys%d" % yb, [lambda e, yb=yb, blk=e_ * NJ + j: e.indirect_dma_start(
                        out=Ys[:, :], out_offset=IOA(ap=idxY[:, blk:blk + 1], axis=0), in_=Yb[yb], in_offset=None,
                        bounds_check=breg(e), oob_is_err=False)], reads=[("Yb", yb, 0), ("Yb", yb, 1)], writes=[("Ys", e_, j)])
                if e_ + 2 < NE:
                    load_part(e_ + 2, 2)
            P.barrier()

            off = base
            NGB = 4
            Yg = []
            for _ in range(2 * NGB):
                a_, off = carve(off, [128, D], F32)
                Yg.append(a_)
            yt0, off = carve(off, [128, D], F32)
            yt1, off = carve(off, [128, D], F32)
            ytt = [yt0, yt1]
            for t in range(NT):
                b = t % 2
                gb = t % NGB
                g0, g1 = Yg[2 * gb], Yg[2 * gb + 1]
                for k, gk in enumerate((g0, g1)):
                    P.dmaf("pool", "ga%d%d" % (gb, k), [lambda e, gk=gk, t=t, k=k: e.indirect_dma_start(
                        out=gk, out_offset=None, in_=Ys[:, :], in_offset=IOA(ap=destg[k][:, t:t + 1], axis=0),
                        bounds_check=breg2(e), oob_is_err=False)], reads=["Ys"], writes=[("Yg", gb, k)])
                P.op("dve", lambda e, b=b, g0=g0, t=t: e.tensor_scalar(out=ytt[b], in0=g0, scalar1=pw1[:, t:t + 1], scalar2=None, op0=ALU.mult),
                     reads=[("Yg", gb, 0)], writes=[("yt", b)])
                P.op("dve", lambda e, b=b, g1=g1, t=t: e.scalar_tensor_tensor(out=ytt[b], in0=g1, scalar=pw2[:, t:t + 1], in1=ytt[b],
                                                                              op0=ALU.mult, op1=ALU.add),
                     reads=[("Yg", gb, 1), ("yt", b)], writes=[("yt", b)])
                for hh in range(2):
                    bank = 2 * b + hh
                    P.mm([lambda e, c=c, b=b, bank=bank: e.transpose(ps[bank][:, (c % 4) * 128:(c % 4 + 1) * 128], ytt[b][:, c * 128:(c + 1) * 128], ident)
                          for c in range(4 * hh, 4 * hh + 4)], reads=[("yt", b), "consts"], writes=[("ps", bank)])
                    P.op("dve", lambda e, hh=hh, t=t, bank=bank: e.tensor_tensor(
                        out=h[:, 4 * hh:4 * hh + 4, t * 128:(t + 1) * 128], in0=h[:, 4 * hh:4 * hh + 4, t * 128:(t + 1) * 128],
                        in1=ps[bank].rearrange("p (c n) -> p c n", n=128), op=ALU.add),
                        reads=[("ps", bank)] + [("h", c, t // 4) for c in range(4 * hh, 4 * hh + 4)],
                        writes=[("h", c, t // 4) for c in range(4 * hh, 4 * hh + 4)])
            P.barrier()

        def ple(l, pre=False):
            off = 0
            wpg, off = carve(off, [128, NCH, D], BF16)
            wpl, off = carve(off, [128, 2, D], BF16)
            pTs, off = carve(off, [128, 2, S], BF16)
            sg0, off = carve(off, [128, TW], F32)
            sg1, off = carve(off, [128, TW], F32)
            sg = [sg0, sg1]
            if pre:
                P.dma("pool", "wP", [(wpg, w_pg[l].rearrange("(kc p) d -> p kc d", p=128)),
                                     (wpl, w_ple[l].rearrange("(kc p) d -> p kc d", p=128)),
                                     (pTs, pT[l].rearrange("(kc p) t -> p kc t", p=128))], writes=["wple"])
                return
            i = 0
            for dc in range(NCH):
                for t in range(TG):
                    q = i % 2
                    ga = (i % 4)
                    pl = 4 + (i % 4)
                    i += 1
                    P.mm([MM(ps[ga], wpg[:, kc, dc * 128:(dc + 1) * 128], xn[:, kc, tsl(t)], kc == 0, kc == NCH - 1)
                          for kc in range(NCH)], reads=["wple"] + XNK, writes=[("ps", ga)])
                    P.mm([MM(ps[pl], wpl[:, kc, dc * 128:(dc + 1) * 128], pTs[:, kc, tsl(t)], kc == 0, kc == 1)
                          for kc in range(2)], reads=["wple"], writes=[("ps", pl)])
                    P.op("act", lambda e, dc=dc, ga=ga, q=q: e.activation(out=sg[q], in_=ps[ga], func=AF.Sigmoid,
                                                                          bias=vcol(V_BPLE + l, dc)),
                         reads=[("ps", ga), "consts"], writes=[("sg", q)])
                    P.op("dve", lambda e, q=q, pl=pl: e.tensor_tensor(out=sg[q], in0=sg[q], in1=ps[pl], op=ALU.mult),
                         reads=[("sg", q), ("ps", pl)], writes=[("sg", q)])
                    P.op("dve", lambda e, dc=dc, t=t, q=q: e.tensor_tensor(out=h[:, dc, tsl(t)], in0=h[:, dc, tsl(t)], in1=sg[q], op=ALU.add),
                         reads=[("sg", q), ("h", dc, t)], writes=[("h", dc, t)])
            P.barrier()

        def forward():
            for l in range(DEPTH):
                if l == 0:
                    mixer_a(pre=True)
                else:
                    mixer_b(pre=True)
                rmsnorm(3 * l + 0)
                if l == 0:
                    mixer_a()
                else:
                    mixer_b()
                if stop_after == ("mix", l):
                    return dump_h()
                router_prep(l)
                rmsnorm(3 * l + 1, tok=True, hook=router_mm)
                moe_sorted(l)
                if stop_after == ("moe", l):
                    return dump_h()
                ple(l, pre=True)
                rmsnorm(3 * l + 2)
                ple(l)
                if stop_after == ("ple", l):
                    return dump_h()
            rmsnorm(6, mode="out")
            P.finish()

        forward()
        P.replay()
    return nc


_NC_CACHE = {}


def _pack_cols(v):
    return np.ascontiguousarray(np.asarray(v, np.float32).reshape(8, 128).T)


def _consts():
    ident = np.eye(128, dtype=np.float32)
    tri = np.triu(np.ones((128, 128), np.float32), k=1)
    ebase = np.tile((np.arange(NE, dtype=np.float32) * CAP)[None, :], (128, 1))
    return np.ascontiguousarray(np.concatenate([ident, tri, ebase], axis=1))


def prepare_shared(inp):
    g = lambda k: np.asarray(inp[k], np.float32)
    blocks = []
    nm, nf, npl = g("norm_mix"), g("norm_ffn"), g("norm_ple")
    for l in range(DEPTH):
        blocks += [nm[l], nf[l], npl[l]]
    blocks.append(g("norm_final"))
    ca = g("conv_a")[0]
    blocks += [ca[0], ca[1], ca[2]]
    cb = g("conv_b")[0]
    blocks += [cb[0], cb[1], cb[2], cb[3]]
    blocks.append(g("conv_bias_b")[0])
    blocks += [g("b_rgate_b")[0][0], g("b_rgate_b")[0][1]]
    blocks += [g("b_igate_b")[0][0], g("b_igate_b")[0][1]]
    blocks += [g("lam_b")[0][0], g("lam_b")[0][1]]
    blocks += [g("b_ple_gate")[0], g("b_ple_gate")[1]]
    assert len(blocks) == NVB
    vecs = np.ascontiguousarray(np.concatenate([_pack_cols(b) for b in blocks], axis=1))
    wrc = np.concatenate([g("w_router_group"), g("w_router_expert")], axis=-1)
    wr = np.ascontiguousarray(wrc.reshape(DEPTH, 8, 128, 36).transpose(2, 0, 1, 3).reshape(128, -1))
    br = np.ascontiguousarray(np.concatenate([g("b_router_group"), g("b_router_expert")], axis=-1).reshape(-1))
    wg = np.stack([g("w_rgate_b")[0], g("w_igate_b")[0]], axis=0)
    wg = wg.reshape(2, 2, 4, 2, 128, 256).transpose(4, 0, 1, 2, 3, 5)
    wg = np.ascontiguousarray(wg.reshape(128, -1))
    shared = {
        "vecs": vecs, "wr": wr, "br": br, "cst": _consts(),
        "w_in_a": np.ascontiguousarray(g("w_in_a")[0]), "w_out_a": np.ascontiguousarray(g("w_out_a")[0]),
        "w_in_b": np.ascontiguousarray(g("w_in_b")[0]), "wgates": wg,
        "w_out_b": np.ascontiguousarray(g("w_out_b")[0]),
        "w_exp_gate": g("w_exp_gate"), "w_exp_up": g("w_exp_up"), "w_exp_down": g("w_exp_down"),
        "w_ple": g("w_ple"), "w_ple_gate": g("w_ple_gate"),
    }
    return shared


def make_in_maps(inp, cores):
    shared = prepare_shared(inp)
    x = np.asarray(inp["x"], np.float32)
    p = np.asarray(inp["p"], np.float32)
    maps = []
    for b in cores:
        m = dict(shared)
        m["xT"] = np.ascontiguousarray(x[b].T)
        m["pT"] = np.ascontiguousarray(p[:, b].transpose(0, 2, 1))
        maps.append(m)
    return maps


def kernel(**inputs):
    key = "full"
    if key not in _NC_CACHE:
        _NC_CACHE[key] = build_program()
    nc = _NC_CACHE[key]
    cores = list(range(8))
    in_maps = make_in_maps(inputs, cores)
    res = run_bass_kernel_spmd(nc, in_maps, core_ids=cores)
    out = np.stack([np.ascontiguousarray(r["yT"].T) for r in res.results], axis=0)
    return out.astype(np.float32)
```

```python
import numpy as np
from contextlib import ExitStack
import concourse.bass as bass
import concourse.mybir as mybir
from concourse.bass_utils import run_bass_kernel_spmd

F32 = mybir.dt.float32
BF16 = mybir.dt.bfloat16
I32 = mybir.dt.int32
AF = mybir.ActivationFunctionType
ALU = mybir.AluOpType
AX = mybir.AxisListType

D = 1024
S = 2048
NCH = 8
TG = 4
TW = 512
NT = 16
NE = 32
DEPTH = 2
EPS = 1e-6
BIG = 1.0e30
CAP = 384
NSLOT = NE * CAP

V_GAIN = 0
V_CONVA = 7
V_CONVB = 10
V_CBIAS = 14
V_BR = 15
V_BI = 17
V_LAM = 19
V_BPLE = 21
NVB = 23

SCR_BYTES = 104 * 1024


class Prog:
    ENG = ("pe", "act", "dve", "pool", "sp")

    def __init__(self, nc, es):
        self.nc = nc
        self.es = es
        self.stream = {e: [] for e in self.ENG}
        self.semh = {}
        self.cnt = {}
        for e in ("pe", "act", "dve", "pool"):
            self._mksem("e:" + e)
        self.known = {e: {} for e in self.ENG}
        self.lastw = {}
        self.readers = {}
        self.ninst = 0
        self.bg_sems = set()
        self.bg_keys = set()

    def require(self, eng, sem):
        s = "d:" + sem
        self._emit_waits(eng, {s: self.cnt[s]})

    def _mksem(self, name):
        if name not in self.semh:
            self.semh[name] = self.es.enter_context(self.nc.semaphore(name.replace(":", "_")))
            self.cnt[name] = 0

    def _deps(self, reads, writes):
        need = {}

        def add(ev):
            s, v = ev
            if v > need.get(s, 0):
                need[s] = v
        for k in reads:
            if k in self.lastw:
                add(self.lastw[k])
        for k in writes:
            if k in self.lastw:
                add(self.lastw[k])
            for s, v in self.readers.get(k, {}).items():
                add((s, v))
        return need

    def _emit_waits(self, eng, need):
        for s, v in need.items():
            if eng == "pe" and s == "e:pe":
                continue
            if self.known[eng].get(s, 0) >= v:
                continue
            self.known[eng][s] = v
            h = self.semh[s]
            self.stream[eng].append(lambda e, h=h, v=v: e.wait_ge(h, v))

    def _commit(self, ev, reads, writes):
        ws = set(writes)
        for k in writes:
            self.lastw[k] = ev
            self.readers[k] = {}
        for k in reads:
            if k in ws:
                continue
            d = self.readers.setdefault(k, {})
            if ev[1] > d.get(ev[0], 0):
                d[ev[0]] = ev[1]

    def op(self, eng, fn, reads=(), writes=()):
        self._emit_waits(eng, self._deps(reads, writes))
        s = "e:" + eng
        self.cnt[s] += 1
        h = self.semh[s]
        self.stream[eng].append(lambda e, fn=fn, h=h: fn(e).then_inc(h, 1))
        self._commit((s, self.cnt[s]), reads, writes)
        self.ninst += 1

    def mm(self, fns, reads=(), writes=()):
        self._emit_waits("pe", self._deps(reads, writes))
        s = "e:pe"
        for f in fns[:-1]:
            self.stream["pe"].append(lambda e, f=f: f(e))
        self.cnt[s] += 1
        h = self.semh[s]
        last = fns[-1]
        self.stream["pe"].append(lambda e, last=last, h=h: last(e).then_inc(h, 1))
        self._commit((s, self.cnt[s]), reads, writes)
        self.ninst += len(fns)

    def dma(self, eng, sem, pairs, reads=(), writes=()):
        s = "d:" + sem
        self._mksem(s)
        self._emit_waits(eng, self._deps(reads, writes))
        h = self.semh[s]
        for (o, i) in pairs:
            self.cnt[s] += 16
            self.stream[eng].append(lambda e, o=o, i=i, h=h: e.dma_start(out=o, in_=i).then_inc(h, 16))
        self._commit((s, self.cnt[s]), reads, writes)
        self.ninst += len(pairs)

    def dmaf(self, eng, sem, fns, reads=(), writes=()):
        s = "d:" + sem
        self._mksem(s)
        self._emit_waits(eng, self._deps(reads, writes))
        h = self.semh[s]
        for f in fns:
            self.cnt[s] += 16
            self.stream[eng].append(lambda e, f=f, h=h: f(e).then_inc(h, 16))
        self._commit((s, self.cnt[s]), reads, writes)
        self.ninst += len(fns)

    def barrier(self, keep=(), skip_sems=()):
        saved = {k: self.lastw[k] for k in list(keep) + list(self.bg_keys) if k in self.lastw}
        skip = set("d:" + x for x in list(skip_sems) + list(self.bg_sems))
        for eng in self.ENG:
            need = {s: v for s, v in self.cnt.items() if v > 0 and s != "e:" + eng and s not in skip}
            if eng != "sp" and eng != "pe" and self.cnt["e:" + eng] > 0:
                need["e:" + eng] = self.cnt["e:" + eng]
            self._emit_waits(eng, need)
        self.lastw = dict(saved)
        self.readers = {}

    def finish(self):
        need = {s: v for s, v in self.cnt.items() if v > 0}
        self._emit_waits("sp", need)

    def replay(self):
        nc = self.nc
        st = self.stream
        with nc.Block() as block:
            @block.tensor
            def _(e):
                for f in st["pe"]:
                    f(e)

            @block.scalar
            def _(e):
                for f in st["act"]:
                    f(e)

            @block.vector
            def _(e):
                for f in st["dve"]:
                    f(e)

            @block.gpsimd
            def _(e):
                for f in st["pool"]:
                    f(e)

            @block.sync
            def _(e):
                for f in st["sp"]:
                    f(e)


def MM(o, l, r, st, sp):
    return lambda e: e.matmul(o, l, r, start=st, stop=sp)


def build_program(stop_after=None, moe_experts=NE):
    nc = bass.Bass("TRN2", target_bir_lowering=False)

    def din(name, shape):
        return nc.dram_tensor(name, list(shape), F32, kind="ExternalInput").ap()

    xT = din("xT", [D, S])
    pT = din("pT", [DEPTH, 256, S])
    vecs_d = din("vecs", [128, NVB * 8])
    wr_d = din("wr", [128, DEPTH * 8 * 36])
    br_d = din("br", [DEPTH * 36])
    cst_d = din("cst", [128, 288])
    w_in_a = din("w_in_a", [D, 3 * D])
    w_out_a = din("w_out_a", [D, D])
    w_in_b = din("w_in_b", [D, 2 * D])
    wg_d = din("wgates", [128, 8192])
    w_out_b = din("w_out_b", [D, D])
    weg = din("w_exp_gate", [DEPTH, NE, 128, 4096])
    weu = din("w_exp_up", [DEPTH, NE, 128, 4096])
    wed = din("w_exp_down", [DEPTH, NE, 128, 4096])
    w_ple = din("w_ple", [DEPTH, 256, D])
    w_pg = din("w_ple_gate", [DEPTH, D, D])
    yT = nc.dram_tensor("yT", [D, S], F32, kind="ExternalOutput").ap()
    Xs = nc.dram_tensor("Xs_scr", [NSLOT, D], BF16, kind="Internal").ap()
    Ys = nc.dram_tensor("Ys_scr", [NSLOT + 128, D], F32, kind="Internal").ap()

    es = ExitStack()
    with es:
        def sb(name, shape, dt):
            return es.enter_context(nc.sbuf_tensor(name, shape, dt))

        h = sb("h", [128, NCH, S], F32)
        xn = sb("xn", [128, NCH, S], BF16)
        scr = sb("scr", [128, SCR_BYTES // 2], BF16)
        vecs = sb("vecs_sb", [128, NVB * 8], F32)
        wr = sb("wr_sb", [128, DEPTH, 8, 36], F32)
        brb = sb("br_sb", [128, DEPTH, 36], F32)
        cst = sb("cst_sb", [128, 288], F32)
        ident = cst[:, 0:128]
        tri = cst[:, 128:256]
        ebase = cst[:, 256:288]
        identb = sb("identb", [128, 128], BF16)
        pw1 = sb("pw1", [128, NT], F32)
        pw2 = sb("pw2", [128, NT], F32)
        desti = [sb("desti0", [128, NT], I32), sb("desti1", [128, NT], I32)]
        destg = [sb("destg0", [128, NT], I32), sb("destg1", [128, NT], I32)]
        ones = sb("ones_sb", [128, 128], F32)
        onesb = sb("onesb_sb", [128, 128], BF16)
        rstd_tok = sb("rstd_tok", [128, NT], F32)
        cneg = sb("cneg", [128, 16], F32)
        cneg2 = sb("cneg2", [128, 16], F32)
        carry = sb("carry", [128, 1], F32)
        hbias = sb("hbias", [128, 32], F32)
        wrs = sb("wrs_p", [128, 8, 36], F32)
        idxY = sb("idxY", [128, NE * (CAP // 128)], I32)
        chh = sb("chh", [128, 16], F32)
        psA = es.enter_context(nc.psum_tensor("psA", [128, 2048], F32))
        psB = es.enter_context(nc.psum_tensor("psB", [128, 2048], F32))
        ps = [psA[:, i * TW:(i + 1) * TW] for i in range(4)] + [psB[:, i * TW:(i + 1) * TW] for i in range(4)]

        P = Prog(nc, es)
        _breg = {}

        def carve(off, shape, dt):
            n = int(np.prod(shape[1:]))
            nb = n * (4 if dt == F32 else 2)
            assert off % 4 == 0 and off + nb <= SCR_BYTES, (off, nb)
            a = scr[:, off // 2: (off + nb) // 2]
            if dt == F32:
                a = a.bitcast(F32)
            if len(shape) == 3:
                a = a.rearrange("p (a b) -> p a b", b=shape[2])
            elif len(shape) == 4:
                a = a.rearrange("p (a b c) -> p a b c", b=shape[2], c=shape[3])
            if shape[0] != 128:
                a = a[0:shape[0]]
            return a, off + nb

        def vcol(blk, c):
            return vecs[:, blk * 8 + c: blk * 8 + c + 1]

        def hk(c):
            return [("h", c, t) for t in range(TG)]

        XNK = [("xn", c) for c in range(NCH)]

        def tsl(t):
            return slice(t * TW, (t + 1) * TW)

        for c in range(NCH):
            P.dma("sp", "ldh%d" % c, [(h[:, c, :], xT[c * 128:(c + 1) * 128, :])], writes=hk(c))
        P.dma("sp", "ldc", [(vecs[:], vecs_d[:, :]),
                            (wr[:].rearrange("p l c j -> p (l c j)"), wr_d[:, :]),
                            (brb[:].rearrange("p l j -> p (l j)"), br_d.partition_broadcast(128)),
                            (cst[:], cst_d[:, :])],
              writes=["consts"])
        P.op("dve", lambda e: e.memset(ones[:], 1.0), writes=["ones"])
        P.op("dve", lambda e: e.memset(onesb[:], 1.0), writes=["onesb"])
        P.op("dve", lambda e: e.tensor_copy(out=identb[:], in_=ident), reads=["consts"], writes=["identb"])
        P.op("act", lambda e: e.activation(out=cneg[:], in_=vecs[:, V_LAM * 8:V_LAM * 8 + 16], func=AF.Exp, scale=-1.0),
             reads=["consts"], writes=["cneg"])
        P.op("act", lambda e: e.activation(out=cneg[:], in_=cneg[:], func=AF.Ln, bias=1.0),
             reads=["cneg"], writes=["cneg"])
        P.op("dve", lambda e: e.tensor_scalar(out=cneg2[:], in0=cneg[:], scalar1=-16.0, scalar2=None, op0=ALU.mult),
             reads=["cneg"], writes=["cneg2"])
        P.op("dve", lambda e: e.tensor_scalar(out=cneg[:], in0=cneg[:], scalar1=-8.0, scalar2=None, op0=ALU.mult),
             reads=["cneg"], writes=["cneg"])
        P.op("dve", lambda e: e.tensor_scalar(out=hbias[:], in0=vecs[:, V_BR * 8:V_BR * 8 + 32], scalar1=0.5, scalar2=None, op0=ALU.mult),
             reads=["consts"], writes=["hbias"])
        P.op("dve", lambda e: e.tensor_scalar(out=chh[:], in0=cneg[:], scalar1=0.5, scalar2=None, op0=ALU.mult),
             reads=["cneg"], writes=["chh"])

        def rmsnorm(idx, mode="bf", tok=False, hook=None):
            off = SCR_BYTES - 24 * 1024
            sq0, off = carve(off, [128, S], F32)
            sq1, off = carve(off, [128, S], F32)
            rstd, off = carve(off, [128, S], F32)
            sq = [sq0, sq1]
            sqb = [sq0.bitcast(BF16)[:, 0:S], sq0.bitcast(BF16)[:, S:2 * S], sq1.bitcast(BF16)[:, 0:S], sq1.bitcast(BF16)[:, S:2 * S]]
            for c in range(NCH):
                b = c % 4
                if c not in (2, 5):
                    P.op("act", lambda e, c=c, b=b: e.activation(out=sqb[b], in_=h[:, c, :], func=AF.Square),
                         reads=hk(c), writes=[("sqb", b)])
                else:
                    P.op("pool", lambda e, c=c, b=b: e.tensor_tensor(out=sqb[b], in0=h[:, c, :], in1=h[:, c, :], op=ALU.mult),
                         reads=hk(c), writes=[("sqb", b)])
                P.mm([MM(ps[t], onesb[:], sqb[b][:, tsl(t)], c == 0, c == NCH - 1) for t in range(TG)],
                     reads=[("sqb", b), "onesb"], writes=[("ps", t) for t in range(TG)])
            for t in range(TG):
                P.op("act", lambda e, t=t: e.activation(out=rstd[:, tsl(t)], in_=ps[t], func=AF.Sqrt,
                                                        scale=1.0 / D, bias=EPS),
                     reads=[("ps", t)], writes=[("rstd", t)])
                P.op("dve", lambda e, t=t: e.reciprocal(out=rstd[:, tsl(t)], in_=rstd[:, tsl(t)]),
                     reads=[("rstd", t)], writes=[("rstd", t)])
            if hook is not None:
                hook()
            RK = [("rstd", t) for t in range(TG)]
            for c in range(NCH):
                if mode == "bf":
                    P.op("dve", lambda e, c=c: e.scalar_tensor_tensor(
                        out=xn[:, c, :], in0=h[:, c, :], scalar=vcol(V_GAIN + idx, c), in1=rstd,
                        op0=ALU.mult, op1=ALU.mult), reads=hk(c) + RK + ["consts"], writes=[("xn", c)])
                else:
                    b = c % 2
                    P.op("dve", lambda e, c=c, b=b: e.scalar_tensor_tensor(
                        out=sq[b], in0=h[:, c, :], scalar=vcol(V_GAIN + idx, c), in1=rstd,
                        op0=ALU.mult, op1=ALU.mult), reads=hk(c) + RK + ["consts"], writes=[("sq", b)])
                    P.dma("sp", "st%d" % b, [(yT[c * 128:(c + 1) * 128, :], sq[b])], reads=[("sq", b)])
            if tok:
                P.mm([MM(ps[4][:, t:t + 1], rstd[:, t * 128:(t + 1) * 128], ident[:, 0:1], True, True)
                      for t in range(NT)], reads=RK + ["consts"], writes=[("ps", 4)])
                P.op("dve", lambda e: e.tensor_copy(out=rstd_tok[:], in_=ps[4][:, 0:NT]),
                     reads=[("ps", 4)], writes=["rstd_tok"])
            P.barrier()

        def dump_h():
            P.dma("sp", "st0", [(yT[c * 128:(c + 1) * 128, :], h[:, c, :]) for c in range(NCH)],
                  reads=[k for c in range(NCH) for k in hk(c)])
            P.finish()

        def mixer_a(pre=False):
            off = 0
            v, off = carve(off, [128, NCH, S], BF16)
            wout, off = carve(off, [128, NCH, D], BF16)
            win0, off = carve(off, [128, 3, NCH, 128], BF16)
            win1, off = carve(off, [128, 3, NCH, 128], BF16)
            csb, off = carve(off, [128, S], F32)
            ch, off = carve(off, [128, S], F32)
            u, off = carve(off, [128, S], F32)
            win = [win0, win1]
            def load_win(c):
                b = c % 2
                pairs = [(win[b][:, part, :, :],
                          w_in_a[:, part * D + c * 128: part * D + (c + 1) * 128].rearrange("(kc p) n -> p kc n", p=128))
                         for part in range(3)]
                P.dma("pool", "wAi%d" % b, pairs, writes=[("win", b)])
            if pre:
                load_win(0)
                load_win(1)
                P.dma("pool", "wAo", [(wout, w_out_a.rearrange("(kc p) d -> p kc d", p=128))], writes=["wout"])
                return
            zt, _ = carve(84 * 1024, [128, 8, D], BF16)
            ztf = zt.rearrange("p a d -> p (a d)").bitcast(F32).rearrange("p (a d) -> p a d", d=D)
            P.op("pool", lambda e: e.memset(zt, 0.0), writes=["zt"])
            zjobs = [("zx", Xs[i * 1024:(i + 1) * 1024, :].rearrange("(j p) d -> p j d", p=128), zt) for i in range(NSLOT // 1024)]
            zjobs += [("zy", Ys[i * 512:(i + 1) * 512, :].rearrange("(j p) d -> p j d", p=128), ztf) for i in range(NSLOT // 512)]
            zjobs += [("zy", Ys[NSLOT:NSLOT + 128, :], ztf[:, 0, :])]

            def zero_some(n, after):
                for _ in range(n):
                    if not zjobs:
                        return
                    sem_, dst_, src_ = zjobs.pop(0)
                    P.dma("sp", sem_, [(dst_, src_)], reads=["zt"] + after, writes=[])
            for c in range(NCH):
                b = c % 2
                if c >= 2:
                    load_win(c)
                zero_some(5, [("v", c - 1, 3)] if c >= 1 else [])
                for (part, base) in ((1, 0), (2, 4)):
                    for t in range(TG):
                        P.mm([MM(ps[base + t], win[b][:, part, kc, :], xn[:, kc, tsl(t)], kc == 0, kc == NCH - 1)
                              for kc in range(NCH)], reads=[("win", b)] + XNK, writes=[("ps", base + t)])
                for t in range(TG):
                    P.op("act", lambda e, t=t: e.activation(out=csb[:, tsl(t)], in_=ps[t], func=AF.Copy),
                         reads=[("ps", t)], writes=[("csb", t)])
                for t in range(TG):
                    P.op("dve", lambda e, t=t: e.tensor_tensor(out=ch[:, tsl(t)], in0=csb[:, tsl(t)], in1=ps[4 + t], op=ALU.mult),
                         reads=[("csb", t), ("ps", 4 + t)], writes=[("ch", t)])
                CHK = [("ch", t) for t in range(TG)]
                P.op("dve", lambda e, c=c: e.tensor_scalar(out=u, in0=ch, scalar1=vcol(V_CONVA + 1, c), scalar2=None, op0=ALU.mult),
                     reads=CHK + ["consts"], writes=["u"])
                P.op("dve", lambda e, c=c: e.scalar_tensor_tensor(out=u[:, 1:S], in0=ch[:, 0:S - 1], scalar=vcol(V_CONVA + 0, c),
                                                                  in1=u[:, 1:S], op0=ALU.mult, op1=ALU.add),
                     reads=CHK + ["u"], writes=["u"])
                P.op("dve", lambda e, c=c: e.scalar_tensor_tensor(out=u[:, 0:S - 1], in0=ch[:, 1:S], scalar=vcol(V_CONVA + 2, c),
                                                                  in1=u[:, 0:S - 1], op0=ALU.mult, op1=ALU.add),
                     reads=CHK + ["u"], writes=["u"])
                for t in range(TG):
                    P.mm([MM(ps[t], win[b][:, 0, kc, :], xn[:, kc, tsl(t)], kc == 0, kc == NCH - 1)
                          for kc in range(NCH)], reads=[("win", b)] + XNK, writes=[("ps", t)])
                for t in range(TG):
                    P.op("dve", lambda e, c=c, t=t: e.tensor_tensor(out=v[:, c, tsl(t)], in0=ps[t], in1=u[:, tsl(t)], op=ALU.mult),
                         reads=[("ps", t), "u"], writes=[("v", c, t)])
            out_proj(v, wout)

        def out_proj(v, wout):
            i = 0
            for dc in range(NCH):
                for t in range(TG):
                    bank = i % 8
                    i += 1
                    P.mm([MM(ps[bank], wout[:, kc, dc * 128:(dc + 1) * 128], v[:, kc, tsl(t)], kc == 0, kc == NCH - 1)
                          for kc in range(NCH)],
                         reads=["wout"] + [("v", kc, t) for kc in range(NCH)], writes=[("ps", bank)])
                    P.op("dve", lambda e, dc=dc, t=t, bank=bank: e.tensor_tensor(
                        out=h[:, dc, tsl(t)], in0=h[:, dc, tsl(t)], in1=ps[bank], op=ALU.add),
                        reads=[("ps", bank), ("h", dc, t)], writes=[("h", dc, t)])
            P.barrier()

        def mixer_b(pre=False):
            off = 0
            v, off = carve(off, [128, NCH, S], BF16)
            wgo, off = carve(off, [128, 8192], BF16)
            win0, off = carve(off, [128, 2, NCH, 128], BF16)
            win1, off = carve(off, [128, 2, NCH, 128], BF16)
            u32a, off = carve(off, [128, S], F32)
            u32b, off = carve(off, [128, S], F32)
            ubfa, off = carve(off, [128, S], BF16)
            ubfb, off = carve(off, [128, S], BF16)
            hacc, off = carve(off, [128, S], F32)
            HW = 1024
            rr, off = carve(off, [128, HW], F32)
            ii, off = carve(off, [128, HW], F32)
            aa, off = carve(off, [128, HW], F32)
            a2, off = carve(off, [128, HW], F32)
            win = [win0, win1]
            u32 = [u32a, u32b]
            ubf = [ubfa, ubfb]
            wg = wgo.rearrange("p (g d h k n) -> p g d h k n", g=2, d=2, h=4, k=2)
            tile_ctr = [0]
            def load_win(c):
                b = c % 2
                pairs = [(win[b][:, part, :, :],
                          w_in_b[:, part * D + c * 128: part * D + (c + 1) * 128].rearrange("(kc p) n -> p kc n", p=128))
                         for part in range(2)]
                P.dma("pool", "wBi%d" % b, pairs, writes=[("win", b)])
            if pre:
                load_win(0)
                load_win(1)
                P.dma("pool", "wBg", [(wgo.rearrange("p (a b) -> p a b", b=2048), wg_d.rearrange("p (a b) -> p a b", b=2048))],
                      writes=["wgo"])
                return
            for hd in range(4):
                for j in range(2):
                    c = 2 * hd + j
                    b = c % 2
                    if c >= 2:
                        load_win(c)
                    for t in range(TG):
                        P.mm([MM(ps[t], win[b][:, 1, kc, :], xn[:, kc, tsl(t)], kc == 0, kc == NCH - 1)
                              for kc in range(NCH)], reads=[("win", b)] + XNK, writes=[("ps", t)])
                    PSA = [("ps", t) for t in range(TG)]
                    uk = ("u32", j)
                    P.op("dve", lambda e, c=c, j=j: e.tensor_scalar(out=u32[j], in0=psA[:, :], scalar1=vcol(V_CONVB + 2, c),
                                                                    scalar2=vcol(V_CBIAS, c), op0=ALU.mult, op1=ALU.add),
                         reads=PSA + ["consts"], writes=[uk])
                    for (k, lo_o, lo_i, n) in ((0, 2, 0, S - 2), (1, 1, 0, S - 1), (3, 0, 1, S - 1)):
                        P.op("dve", lambda e, c=c, j=j, k=k, lo_o=lo_o, lo_i=lo_i, n=n: e.scalar_tensor_tensor(
                            out=u32[j][:, lo_o:lo_o + n], in0=psA[:, lo_i:lo_i + n], scalar=vcol(V_CONVB + k, c),
                            in1=u32[j][:, lo_o:lo_o + n], op0=ALU.mult, op1=ALU.add),
                            reads=PSA + [uk], writes=[uk])
                    P.op("act", lambda e, j=j: e.activation(out=ubf[j], in_=u32[j], func=AF.Copy),
                         reads=[uk], writes=[("ubf", j)])
                    for t in range(TG):
                        P.mm([MM(ps[4 + t], win[b][:, 0, kc, :], xn[:, kc, tsl(t)], kc == 0, kc == NCH - 1)
                              for kc in range(NCH)], reads=[("win", b)] + XNK, writes=[("ps", 4 + t)])
                        P.op("act", lambda e, c=c, t=t: e.activation(out=v[:, c, tsl(t)], in_=ps[4 + t], func=AF.Gelu_apprx_tanh),
                             reads=[("ps", 4 + t)], writes=[("v", c, t)])
                for j in range(2):
                    c = 2 * hd + j
                    for dr in range(2):
                        halves = (0, 1) if dr == 0 else (1, 0)
                        for hi, hf in enumerate(halves):
                            hs = slice(hf * HW, (hf + 1) * HW)
                            pb = 4 if (tile_ctr[0] % 2 == 0) else 0
                            tile_ctr[0] += 1
                            psG = psB if pb == 4 else psA
                            for (g, base) in ((0, pb), (1, pb + 2)):
                                for tt in range(2):
                                    t = hf * 2 + tt
                                    P.mm([MM(ps[base + tt], wg[:, g, dr, hd, k, j * 128:(j + 1) * 128], ubf[k][:, tsl(t)], k == 0, k == 1)
                                          for k in range(2)], reads=["wgo", ("ubf", 0), ("ubf", 1)], writes=[("ps", base + tt)])
                            col = dr * 8 + c
                            P.op("act", lambda e, col=col, psG=psG: e.activation(out=rr, in_=psG[:, 0:HW], func=AF.Tanh, scale=0.5,
                                                                                 bias=hbias[:, col:col + 1]),
                                 reads=[("ps", pb), ("ps", pb + 1), "hbias"], writes=["rr"])
                            P.op("act", lambda e, col=col, psG=psG: e.activation(out=ii, in_=psG[:, HW:2 * HW], func=AF.Tanh, scale=0.5,
                                                                                 bias=hbias[:, 16 + col:16 + col + 1]),
                                 reads=[("ps", pb + 2), ("ps", pb + 3), "hbias"], writes=["ii"])
                            P.op("act", lambda e, col=col: e.activation(out=aa, in_=rr, func=AF.Exp, scale=chh[:, col:col + 1],
                                                                        bias=chh[:, col:col + 1]),
                                 reads=["rr", "chh"], writes=["aa"])
                            P.op("act", lambda e, col=col: e.activation(out=a2, in_=rr, func=AF.Exp, scale=cneg[:, col:col + 1],
                                                                        bias=cneg[:, col:col + 1]),
                                 reads=["rr", "cneg"], writes=["a2"])
                            P.op("act", lambda e: e.activation(out=a2, in_=a2, func=AF.Sqrt, scale=-1.0, bias=1.0),
                                 reads=["a2"], writes=["a2"])
                            P.op("dve", lambda e, j=j, hs=hs: e.scalar_tensor_tensor(out=ii, in0=ii, scalar=1.0, in1=u32[j][:, hs],
                                                                                     op0=ALU.add, op1=ALU.mult),
                                 reads=["ii", ("u32", j)], writes=["ii"])
                            P.op("dve", lambda e: e.scalar_tensor_tensor(out=ii, in0=ii, scalar=0.5, in1=a2, op0=ALU.mult, op1=ALU.mult),
                                 reads=["ii", "a2"], writes=["ii"])
                            if dr == 0:
                                if hi == 0:
                                    init = 0.0
                                    rd = []
                                else:
                                    init = hacc[:, HW - 1:HW]
                                    rd = [("hacc", 0)]
                                P.op("dve", lambda e, hs=hs, init=init: e.tensor_tensor_scan(
                                    out=hacc[:, hs], data0=aa, data1=ii, initial=init, op0=ALU.mult, op1=ALU.add),
                                    reads=["aa", "ii"] + rd, writes=[("hacc", hf)])
                            else:
                                if hi == 0:
                                    init = 0.0
                                    rd = []
                                else:
                                    init = carry[:, 0:1]
                                    rd = ["carry"]
                                P.op("dve", lambda e, init=init: e.tensor_tensor_scan(
                                    out=a2[:, ::-1], data0=aa[:, ::-1], data1=ii[:, ::-1], initial=init,
                                    op0=ALU.mult, op1=ALU.add), reads=["aa", "ii", "a2"] + rd, writes=["a2"])
                                if hi == 0:
                                    P.op("dve", lambda e: e.tensor_copy(out=carry[:], in_=a2[:, 0:1]), reads=["a2"], writes=["carry"])
                                P.op("dve", lambda e, hs=hs: e.tensor_tensor(out=hacc[:, hs], in0=hacc[:, hs], in1=a2, op=ALU.add),
                                     reads=["a2", ("hacc", hf)], writes=[("hacc", hf)])
                    for t in range(TG):
                        P.op("dve", lambda e, c=c, t=t: e.tensor_tensor(out=v[:, c, tsl(t)], in0=v[:, c, tsl(t)], in1=hacc[:, tsl(t)], op=ALU.mult),
                             reads=[("v", c, t), ("hacc", t // 2)], writes=[("v", c, t)])
            wout = wgo.rearrange("p (kc d) -> p kc d", d=D)
            P.dma("pool", "wBg", [(wout, w_out_b.rearrange("(kc p) d -> p kc d", p=128))], writes=["wgo", "wout"])
            out_proj(v, wout)

        def moe(l):
            off = 0
            ar0, off = carve(off, [128, 12288], BF16)
            ar1, off = carve(off, [128, 12288], BF16)
            Hs0, off = carve(off, [128, 4, TW], BF16)
            Hs1, off = carve(off, [128, 4, TW], BF16)
            sg0, off = carve(off, [128, TW], F32)
            sg1, off = carve(off, [128, TW], F32)
            tt0, off = carve(off, [128, TW], F32)
            tt1, off = carve(off, [128, TW], F32)
            WT, off = carve(off, [128, S], F32)
            sel, off = carve(off, [128, NE, 128], F32)
            wrs, off = carve(off, [128, 8, 36], F32)
            L, off = carve(off, [128, NT, 36], F32)
            lm, off = carve(off, [128, NT, 32], F32)
            oh1, off = carve(off, [128, NT, 32], F32)
            oh2, off = carve(off, [128, NT, 32], F32)
            Wt, off = carve(off, [128, NT, 32], F32)
            g4a, off = carve(off, [128, NT, 4], F32)
            g4b, off = carve(off, [128, NT, 4], F32)
            s16 = []
            for _ in range(6):
                a, off = carve(off, [128, NT], F32)
                s16.append(a)
            gmax, gsum, m1, m2, pw1, pw2 = s16
            ar = [ar0, ar1]
            Hs = [Hs0, Hs1]
            sg = [sg0, sg1]
            ttm = [tt0, tt1]
            n_exp = moe_experts

            def load_expert(e_):
                b = e_ % 2
                a = ar[b]
                pairs = [(a[:, 0:4096].rearrange("p (kc f) -> p kc f", f=512), weg[l, e_].rearrange("(kc p) f -> p kc f", p=128)),
                         (a[:, 4096:8192].rearrange("p (kc f) -> p kc f", f=512), weu[l, e_].rearrange("(kc p) f -> p kc f", p=128)),
                         (a[:, 8192:12288].rearrange("p (fc d) -> p fc d", d=D), wed[l, e_].rearrange("(fc p) d -> p fc d", p=128))]
                P.dma("pool", "wE%d" % b, pairs, writes=[("ar", b)])

            load_expert(0)
            if n_exp > 1:
                load_expert(1)

            for c in range(NCH):
                P.op("dve", lambda e, c=c: e.tensor_scalar(out=wrs[:, c, :], in0=wr[:, l, c, :], scalar1=vcol(V_GAIN + 3 * l + 1, c),
                                                           scalar2=None, op0=ALU.mult), reads=["consts"], writes=["wrs"])
            P.op("dve", lambda e: e.tensor_copy(out=sel[0:32], in_=cst[0:32, 0:32].unsqueeze(2).to_broadcast([32, 32, 128])),
                 reads=["consts"], writes=["sel"])
            for t in range(NT):
                bank = t % 4
                P.mm([MM(ps[bank][:, 0:36], h[:, c, t * 128:(t + 1) * 128], wrs[:, c, :], c == 0, c == NCH - 1) for c in range(NCH)],
                     reads=["wrs"] + [("h", c, t // 4) for c in range(NCH)], writes=[("ps", bank)])
                P.op("dve", lambda e, t=t, bank=bank: e.scalar_tensor_tensor(
                    out=L[:, t, :], in0=ps[bank][:, 0:36], scalar=rstd_tok[:, t:t + 1], in1=brb[:, l, :],
                    op0=ALU.mult, op1=ALU.add), reads=[("ps", bank), "rstd_tok", "consts"], writes=["L"])
            gl = L[:, :, 0:4]
            le4 = L[:, :, 4:36].rearrange("p t (g e) -> p t g e", e=8)
            lm4 = lm.rearrange("p t (g e) -> p t g e", e=8)

            def bc3(a, n):
                return a.unsqueeze(2).to_broadcast([128, NT, n])
            R = "route"
            P.op("dve", lambda e: e.tensor_reduce(out=gmax, in_=gl, axis=AX.X, op=ALU.max), reads=["L"], writes=[R])
            P.op("dve", lambda e: e.tensor_tensor(out=g4a, in0=gl, in1=bc3(gmax, 4), op=ALU.is_equal), reads=["L", R], writes=[R])
            P.op("dve", lambda e: e.tensor_tensor(out=g4b, in0=gl, in1=bc3(gmax, 4), op=ALU.subtract), reads=["L", R], writes=[R])
            P.op("act", lambda e: e.activation(out=g4b, in_=g4b, func=AF.Exp), reads=[R], writes=[R])
            P.op("dve", lambda e: e.tensor_reduce(out=gsum, in_=g4b, axis=AX.X, op=ALU.add), reads=[R], writes=[R])
            P.op("dve", lambda e: e.reciprocal(out=gsum, in_=gsum), reads=[R], writes=[R])
            P.op("dve", lambda e: e.tensor_scalar(out=g4a, in0=g4a, scalar1=-1.0, scalar2=BIG, op0=ALU.add, op1=ALU.mult),
                 reads=[R], writes=[R])
            P.op("dve", lambda e: e.tensor_tensor(out=lm4, in0=le4, in1=g4a.unsqueeze(3).to_broadcast([128, NT, 4, 8]), op=ALU.add),
                 reads=["L", R], writes=[R])
            P.op("dve", lambda e: e.tensor_reduce(out=m1, in_=lm, axis=AX.X, op=ALU.max), reads=[R], writes=[R])
            P.op("dve", lambda e: e.tensor_tensor(out=oh1, in0=lm, in1=bc3(m1, 32), op=ALU.is_equal), reads=[R], writes=[R])
            P.op("dve", lambda e: e.scalar_tensor_tensor(out=lm, in0=oh1, scalar=-BIG, in1=lm, op0=ALU.mult, op1=ALU.add),
                 reads=[R], writes=[R])
            P.op("dve", lambda e: e.tensor_reduce(out=m2, in_=lm, axis=AX.X, op=ALU.max), reads=[R], writes=[R])
            P.op("dve", lambda e: e.tensor_tensor(out=oh2, in0=lm, in1=bc3(m2, 32), op=ALU.is_equal), reads=[R], writes=[R])
            P.op("dve", lambda e: e.tensor_tensor(out=m1, in0=m1, in1=m2, op=ALU.subtract), reads=[R], writes=[R])
            P.op("act", lambda e: e.activation(out=m1, in_=m1, func=AF.Sigmoid), reads=[R], writes=[R])
            P.op("dve", lambda e: e.tensor_tensor(out=pw1, in0=gsum, in1=m1, op=ALU.mult), reads=[R], writes=[R])
            P.op("dve", lambda e: e.tensor_tensor(out=pw2, in0=gsum, in1=pw1, op=ALU.subtract), reads=[R], writes=[R])
            P.op("dve", lambda e: e.tensor_tensor(out=Wt, in0=oh1, in1=bc3(pw1, 32), op=ALU.mult), reads=[R], writes=[R])
            P.op("dve", lambda e: e.tensor_tensor(out=oh2, in0=oh2, in1=bc3(pw2, 32), op=ALU.mult), reads=[R], writes=[R])
            P.op("dve", lambda e: e.tensor_tensor(out=Wt, in0=Wt, in1=oh2, op=ALU.add), reads=[R], writes=[R])
            for t4 in range(TG):
                P.mm([lambda e, t=t4 * 4 + q, q=q, t4=t4: e.transpose(ps[t4][0:32, q * 128:(q + 1) * 128], Wt[:, t, :], ident)
                      for q in range(4)], reads=[R, "consts"], writes=[("ps", t4)])
                P.op("act", lambda e, t4=t4: e.activation(out=WT[0:32, tsl(t4)], in_=ps[t4][0:32, :], func=AF.Copy),
                     reads=[("ps", t4)], writes=[("WT", t4)])

            def GU(step):
                e_, t = divmod(step, TG)
                b = e_ % 2
                a = ar[b]
                hb = step % 2
                wbk = 6 + hb
                P.mm([MM(ps[wbk], sel[0:32, e_, :], WT[0:32, tsl(t)], True, True)],
                     reads=["sel", ("WT", t)], writes=[("ps", wbk)])
                for fc in range(4):
                    q = fc % 2
                    P.mm([MM(ps[q], a[:, kc * 512 + fc * 128: kc * 512 + (fc + 1) * 128], xn[:, kc, tsl(t)], kc == 0, kc == NCH - 1)
                          for kc in range(NCH)], reads=[("ar", b)] + XNK, writes=[("ps", q)])
                    P.mm([MM(ps[2 + q], a[:, 4096 + kc * 512 + fc * 128: 4096 + kc * 512 + (fc + 1) * 128], xn[:, kc, tsl(t)], kc == 0, kc == NCH - 1)
                          for kc in range(NCH)], reads=[("ar", b)] + XNK, writes=[("ps", 2 + q)])
                    P.op("act", lambda e, q=q: e.activation(out=sg[q], in_=ps[q], func=AF.Silu),
                         reads=[("ps", q)], writes=[("sg", q)])
                    P.op("dve", lambda e, q=q: e.tensor_tensor(out=ttm[q], in0=sg[q], in1=ps[2 + q], op=ALU.mult),
                         reads=[("sg", q), ("ps", 2 + q)], writes=[("tt", q)])
                    P.op("dve", lambda e, q=q, hb=hb, fc=fc, wbk=wbk: e.tensor_tensor(out=Hs[hb][:, fc, :], in0=ttm[q], in1=ps[wbk], op=ALU.mult),
                         reads=[("tt", q), ("ps", wbk)], writes=[("Hs", hb, fc)])

            def YY(step):
                e_, t = divmod(step, TG)
                b = e_ % 2
                a = ar[b]
                hb = step % 2
                for dc in range(NCH):
                    yb = 4 + dc % 2
                    P.mm([MM(ps[yb], a[:, 8192 + fc * D + dc * 128: 8192 + fc * D + (dc + 1) * 128], Hs[hb][:, fc, :], fc == 0, fc == 3)
                          for fc in range(4)],
                         reads=[("ar", b)] + [("Hs", hb, fc) for fc in range(4)], writes=[("ps", yb)])
                    P.op("dve", lambda e, dc=dc, t=t, yb=yb: e.tensor_tensor(out=h[:, dc, tsl(t)], in0=h[:, dc, tsl(t)], in1=ps[yb], op=ALU.add),
                         reads=[("ps", yb), ("h", dc, t)], writes=[("h", dc, t)])

            nsteps = n_exp * TG
            for step in range(nsteps + 1):
                if step < nsteps:
                    GU(step)
                if step >= 1:
                    YY(step - 1)
                e_, t = divmod(step, TG)
                if step < nsteps and t == 0 and e_ >= 1 and e_ + 1 < n_exp:
                    load_expert(e_ + 1)
            P.barrier()

        def psR(t):
            return ps[t // 8][:, (t % 8) * 36:(t % 8) * 36 + 36]

        def router_prep(l):
            for c in range(NCH):
                P.op("dve", lambda e, c=c: e.tensor_scalar(out=wrs[:, c, :], in0=wr[:, l, c, :], scalar1=vcol(V_GAIN + 3 * l + 1, c),
                                                           scalar2=None, op0=ALU.mult), reads=["consts"], writes=["wrs"])

        def router_mm():
            off_ = SCR_BYTES - 24 * 1024
            LT, _ = carve(off_, [128, S], F32)
            for t4 in range(TG):
                P.mm([MM(ps[4 + t4][0:36, :], wrs[:, c, :], h[:, c, tsl(t4)], c == 0, c == NCH - 1) for c in range(NCH)],
                     reads=["wrs"] + [("h", c, t4) for c in range(NCH)], writes=[("ps", 4 + t4)])
            for t4 in range(TG):
                P.op("act", lambda e, t4=t4: e.activation(out=LT[0:36, tsl(t4)], in_=ps[4 + t4][0:36, :], func=AF.Copy),
                     reads=[("ps", 4 + t4)], writes=[("LT", t4)] + ([("sqb", 0), ("sqb", 1)] if t4 < 2 else []))
            for t in range(NT):
                P.mm([lambda e, t=t: e.transpose(psR(t), LT[0:36, t * 128:(t + 1) * 128], ident[0:36, 0:36])],
                     reads=[("LT", t // 4), "consts"], writes=[("ps", t // 8)])

        def moe_sorted(l):
            IOA = bass.IndirectOffsetOnAxis
            off = 0

            def breg(e):
                if 'r' not in _breg:
                    _breg['r'] = e.to_reg(NSLOT - 1)
                return _breg['r']

            def breg2(e):
                if 'r2' not in _breg:
                    _breg['r2'] = e.to_reg(NSLOT + 127)
                return _breg['r2']
            ar0, off = carve(off, [128, 12288], BF16)
            ar1, off = carve(off, [128, 12288], BF16)
            base = off
            ar = [ar0, ar1]

            def load_part(e_, part):
                b = e_ % 2
                a = ar[b]
                src = (weg, weu, wed)[part][l, e_]
                pr = (a[:, part * 4096:(part + 1) * 4096].rearrange("p (a b) -> p a b", b=2048), src.rearrange("p (a b) -> p a b", b=2048))
                P.dma("pool", "wE%d%d" % (b, part), [pr], writes=[("ar", b, part)])

            for e0 in range(2):
                for part in range(3):
                    load_part(e0, part)
            ARK = [("ar", b_, p_) for b_ in range(2) for p_ in range(3)]
            ARS = ["wE%d%d" % (b_, p_) for b_ in range(2) for p_ in range(3)]

            off = base
            xt, off = carve(off, [128, NT, D], BF16)
            L, off = carve(off, [128, NT, 36], F32)
            lm, off = carve(off, [128, NT, 32], F32)
            oh1, off = carve(off, [128, NT, 32], F32)
            oh2, off = carve(off, [128, NT, 32], F32)
            Cc, off = carve(off, [128, NT, 32], F32)
            Ccum, off = carve(off, [128, NT, 32], F32)
            Rr, off = carve(off, [128, NT, 32], F32)
            g4a, off = carve(off, [128, NT, 4], F32)
            g4b, off = carve(off, [128, NT, 4], F32)
            s16 = []
            for _ in range(8):
                a_, off = carve(off, [128, NT], F32)
                s16.append(a_)
            gmax, gsum, m1, m2, dstf, rkf, ovf, _sp = s16

            for t in range(NT):
                P.op("dve", lambda e, t=t: e.scalar_tensor_tensor(
                    out=L[:, t, :], in0=psR(t), scalar=rstd_tok[:, t:t + 1], in1=brb[:, l, :],
                    op0=ALU.mult, op1=ALU.add), reads=[("ps", t // 8), "rstd_tok", "consts"], writes=["L"])
            for t in range(NT):
                bank = 4 + t % 4
                psb = ps[bank].bitcast(BF16)
                P.mm([lambda e, c=c, t=t, psb=psb: e.transpose(psb[:, c * 128:(c + 1) * 128], xn[:, c, t * 128:(t + 1) * 128], identb[:])
                      for c in range(NCH)], reads=XNK + ["identb"], writes=[("ps", bank)])
                if t % 2 == 0:
                    P.op("act", lambda e, t=t, psb=psb: e.activation(out=xt[:, t, :], in_=psb, func=AF.Copy),
                         reads=[("ps", bank)], writes=[("xt", t)])
                else:
                    P.op("dve", lambda e, t=t, psb=psb: e.tensor_copy(out=xt[:, t, :], in_=psb),
                         reads=[("ps", bank)], writes=[("xt", t)])
            gl = L[:, :, 0:4]
            le4 = L[:, :, 4:36].rearrange("p t (g e) -> p t g e", e=8)
            lm4 = lm.rearrange("p t (g e) -> p t g e", e=8)

            def bc3(a, n):
                return a.unsqueeze(2).to_broadcast([128, NT, n])
            R = "route"
            P.op("dve", lambda e: e.tensor_reduce(out=gmax, in_=gl, axis=AX.X, op=ALU.max), reads=["L"], writes=[R])
            P.op("dve", lambda e: e.tensor_tensor(out=g4a, in0=gl, in1=bc3(gmax, 4), op=ALU.is_equal), reads=["L", R], writes=[R])
            P.op("dve", lambda e: e.tensor_tensor(out=g4b, in0=gl, in1=bc3(gmax, 4), op=ALU.subtract), reads=["L", R], writes=[R])
            P.op("act", lambda e: e.activation(out=g4b, in_=g4b, func=AF.Exp), reads=[R], writes=[R])
            P.op("dve", lambda e: e.tensor_reduce(out=gsum, in_=g4b, axis=AX.X, op=ALU.add), reads=[R], writes=[R])
            P.op("dve", lambda e: e.reciprocal(out=gsum, in_=gsum), reads=[R], writes=[R])
            P.op("dve", lambda e: e.tensor_scalar(out=g4a, in0=g4a, scalar1=-1.0, scalar2=BIG, op0=ALU.add, op1=ALU.mult),
                 reads=[R], writes=[R])
            P.op("dve", lambda e: e.tensor_tensor(out=lm4, in0=le4, in1=g4a.unsqueeze(3).to_broadcast([128, NT, 4, 8]), op=ALU.add),
                 reads=["L", R], writes=[R])
            P.op("dve", lambda e: e.tensor_reduce(out=m1, in_=lm, axis=AX.X, op=ALU.max), reads=[R], writes=[R])
            P.op("dve", lambda e: e.tensor_tensor(out=oh1, in0=lm, in1=bc3(m1, 32), op=ALU.is_equal), reads=[R], writes=[R])
            P.op("dve", lambda e: e.scalar_tensor_tensor(out=lm, in0=oh1, scalar=-BIG, in1=lm, op0=ALU.mult, op1=ALU.add),
                 reads=[R], writes=[R])
            P.op("dve", lambda e: e.tensor_reduce(out=m2, in_=lm, axis=AX.X, op=ALU.max), reads=[R], writes=[R])
            P.op("dve", lambda e: e.tensor_tensor(out=oh2, in0=lm, in1=bc3(m2, 32), op=ALU.is_equal), reads=[R], writes=[R])
            P.op("dve", lambda e: e.tensor_tensor(out=m1, in0=m1, in1=m2, op=ALU.subtract), reads=[R], writes=[R])
            P.op("act", lambda e: e.activation(out=m1, in_=m1, func=AF.Sigmoid), reads=[R], writes=[R])
            P.op("dve", lambda e: e.tensor_tensor(out=pw1[:], in0=gsum, in1=m1, op=ALU.mult), reads=[R], writes=[R, "pw"])
            P.op("dve", lambda e: e.tensor_tensor(out=pw2[:], in0=gsum, in1=pw1[:], op=ALU.subtract), reads=[R], writes=[R, "pw"])
            P.op("dve", lambda e: e.tensor_tensor(out=Cc, in0=oh1, in1=oh2, op=ALU.add), reads=[R], writes=[R])
            P.op("dve", lambda e: e.memset(Ccum[:, 0, :], 0.0), reads=[R], writes=[R])
            for t in range(1, NT):
                P.op("dve", lambda e, t=t: e.tensor_tensor(out=Ccum[:, t, :], in0=Ccum[:, t - 1, :], in1=Cc[:, t - 1, :], op=ALU.add),
                     reads=[R], writes=[R])
            NB = NE * (CAP // 128)
            rowb, off = carve(off, [128, NB], F32)
            thr, off = carve(off, [128, NB], F32)
            vv, off = carve(off, [128, NB], F32)
            P.op("pool", lambda e: e.iota(rowb, pattern=[[128, NB]], base=0, channel_multiplier=1, allow_small_or_imprecise_dtypes=True),
                 writes=["rowb"])
            P.op("pool", lambda e: e.iota(thr, pattern=[[0, NE], [128, CAP // 128]], base=0, channel_multiplier=1,
                                          allow_small_or_imprecise_dtypes=True), writes=["thr"])
            P.op("dve", lambda e: e.tensor_tensor(out=lm[:, 0, :], in0=Ccum[:, NT - 1, :], in1=Cc[:, NT - 1, :], op=ALU.add),
                 reads=[R], writes=[R])
            P.mm([MM(ps[1][:, 0:NE], ones[:], lm[:, 0, :], True, True)], reads=[R, "ones"], writes=[("ps", 1)])
            P.op("dve", lambda e: e.tensor_tensor(out=vv.rearrange("p (e j) -> p e j", j=CAP // 128),
                                                  in0=thr.rearrange("p (e j) -> p e j", j=CAP // 128),
                                                  in1=ps[1][:, 0:NE].unsqueeze(2).to_broadcast([128, NE, CAP // 128]), op=ALU.is_lt),
                 reads=[("ps", 1), "thr"], writes=["vv"])
            P.op("dve", lambda e: e.tensor_scalar(out=vv, in0=vv, scalar1=-1.0, scalar2=-1.0e6, op0=ALU.add, op1=ALU.mult),
                 reads=["vv"], writes=["vv"])
            P.op("dve", lambda e: e.tensor_tensor(out=vv, in0=vv, in1=rowb, op=ALU.add), reads=["vv", "rowb"], writes=["vv"])
            P.op("dve", lambda e: e.tensor_copy(out=idxY[:], in_=vv), reads=["vv"], writes=["idxY"])
            Cf = Cc.rearrange("p t e -> p (t e)")
            Ccf = Ccum.rearrange("p t e -> p (t e)")
            P.mm([MM(ps[0], tri, Cf, True, False), MM(ps[0], ones[:], Ccf, False, True)],
                 reads=[R, "consts", "ones"], writes=[("ps", 0)])
            rk3 = ps[0].rearrange("p (t e) -> p t e", e=32)
            P.op("dve", lambda e: e.tensor_tensor(out=Rr, in0=rk3, in1=ebase.unsqueeze(1).to_broadcast([128, NT, 32]), op=ALU.add),
                 reads=[("ps", 0), "consts"], writes=[R])
            for k, oh in enumerate((oh1, oh2)):
                P.op("dve", lambda e, oh=oh: e.tensor_tensor(out=lm, in0=oh, in1=Rr, op=ALU.mult), reads=[R], writes=[R])
                P.op("dve", lambda e: e.tensor_reduce(out=dstf, in_=lm, axis=AX.X, op=ALU.add), reads=[R], writes=[R])
                P.op("dve", lambda e, oh=oh: e.tensor_tensor(out=lm, in0=oh, in1=rk3, op=ALU.mult), reads=[R, ("ps", 0)], writes=[R])
                P.op("dve", lambda e: e.tensor_reduce(out=rkf, in_=lm, axis=AX.X, op=ALU.add), reads=[R], writes=[R])
                P.op("dve", lambda e: e.tensor_scalar(out=ovf, in0=rkf, scalar1=float(CAP), scalar2=1.0e6, op0=ALU.is_ge, op1=ALU.mult),
                     reads=[R], writes=[R])
                P.op("dve", lambda e: e.tensor_tensor(out=dstf, in0=dstf, in1=ovf, op=ALU.add), reads=[R], writes=[R])
                P.op("dve", lambda e, k=k: e.tensor_copy(out=desti[k][:], in_=dstf), reads=[R], writes=[R, ("desti", k)])
                P.op("dve", lambda e: e.tensor_scalar(out=dstf, in0=dstf, scalar1=float(NSLOT), scalar2=None, op0=ALU.min), reads=[R], writes=[R])
                P.op("dve", lambda e, k=k: e.tensor_copy(out=destg[k][:], in_=dstf), reads=[R], writes=[R, ("destg", k)])
            fns = []
            for t in range(NT):
                for k in range(2):
                    fns.append(lambda e, t=t, k=k: e.indirect_dma_start(
                        out=Xs[:, :], out_offset=IOA(ap=desti[k][:, t:t + 1], axis=0), in_=xt[:, t, :], in_offset=None,
                        bounds_check=breg(e), oob_is_err=False))
            P.dmaf("pool", "sc", fns, reads=[("xt", t) for t in range(NT)] + [("desti", 0), ("desti", 1)], writes=["Xs"])
            P.barrier(keep=ARK, skip_sems=ARS)

            off = base
            Xb0, off = carve(off, [128, 3, D], BF16)
            Xb1, off = carve(off, [128, 3, D], BF16)
            XT0, off = carve(off, [128, NCH, CAP], BF16)
            XT1, off = carve(off, [128, NCH, CAP], BF16)
            Hs0, off = carve(off, [128, 4, CAP], BF16)
            Hs1, off = carve(off, [128, 4, CAP], BF16)
            sg0, off = carve(off, [128, CAP], F32)
            sg1, off = carve(off, [128, CAP], F32)
            Yb = []
            for _ in range(3):
                a_, off = carve(off, [128, D], F32)
                Yb.append(a_)
            Xb = [Xb0, Xb1]
            XT = [XT0, XT1]
            Hs = [Hs0, Hs1]
            sg = [sg0, sg1]
            NJ = CAP // 128
            def xb_load(e_):
                b = e_ % 2
                P.dmaf("pool", "xb%d" % b, [lambda e, j=j, b=b, e_=e_: e.indirect_dma_start(
                    out=Xb[b][:, j, :], out_offset=None, in_=Xs[:, :],
                    in_offset=IOA(ap=idxY[:, e_ * NJ + j:e_ * NJ + j + 1], axis=0), bounds_check=breg(e), oob_is_err=False)
                    for j in range(NJ)], reads=["Xs"], writes=[("Xb", b)])

            def do_transposes(e_):
                b = e_ % 2
                for j in range(NJ):
                    tb = 6 + (e_ * NJ + j) % 2
                    psb = ps[tb].bitcast(BF16)
                    P.mm([lambda e, c=c, j=j, b=b, psb=psb: e.transpose(psb[:, c * 128:(c + 1) * 128], Xb[b][:, j, c * 128:(c + 1) * 128], identb[:])
                          for c in range(NCH)], reads=[("Xb", b), "identb"], writes=[("ps", tb)])
                    P.op("act", lambda e, j=j, b=b, psb=psb: e.activation(
                        out=XT[b][:, :, j * 128:(j + 1) * 128], in_=psb.rearrange("p (c n) -> p c n", n=128), func=AF.Copy),
                        reads=[("ps", tb)], writes=[("XT", b, j)])

            for b_ in range(2):
                P.op("dve", lambda e, b_=b_: e.memset(Xb[b_], 0.0), writes=[("Xb", b_)])
            xb_load(0)
            xb_load(1)
            do_transposes(0)
            for e_ in range(NE):
                b = e_ % 2
                a = ar[b]
                XTK = [("XT", b, j) for j in range(NJ)]
                for fc in range(4):
                    q = fc % 2
                    P.mm([MM(ps[q][:, 0:CAP], a[:, kc * 512 + fc * 128: kc * 512 + (fc + 1) * 128], XT[b][:, kc, :], kc == 0, kc == NCH - 1)
                          for kc in range(NCH)], reads=[("ar", b, 0)] + XTK, writes=[("ps", q)])
                    P.mm([MM(ps[2 + q][:, 0:CAP], a[:, 4096 + kc * 512 + fc * 128: 4096 + kc * 512 + (fc + 1) * 128], XT[b][:, kc, :], kc == 0, kc == NCH - 1)
                          for kc in range(NCH)], reads=[("ar", b, 1)] + XTK, writes=[("ps", 2 + q)])
                    P.op("act", lambda e, q=q: e.activation(out=sg[q], in_=ps[q][:, 0:CAP], func=AF.Silu),
                         reads=[("ps", q)], writes=[("sg", q)])
                    P.op("dve", lambda e, q=q, b=b, fc=fc: e.tensor_tensor(out=Hs[b][:, fc, :], in0=sg[q], in1=ps[2 + q][:, 0:CAP], op=ALU.mult),
                         reads=[("sg", q), ("ps", 2 + q)], writes=[("Hs", b, fc)])
                if e_ + 2 < NE:
                    load_part(e_ + 2, 0)
                    load_part(e_ + 2, 1)
                if e_ + 1 < NE:
                    do_transposes(e_ + 1)
                if e_ + 2 < NE:
                    xb_load(e_ + 2)
                HK = [("Hs", b, fc) for fc in range(4)]
                for j in range(NJ):
                    yb = (e_ * NJ + j) % 3
                    for half in range(2):
                        bank = 4 + half
                        P.mm([MM(ps[bank], Hs[b][:, fc, j * 128:(j + 1) * 128], a[:, 8192 + fc * D + half * 512: 8192 + fc * D + (half + 1) * 512], fc == 0, fc == 3)
                              for fc in range(4)], reads=[("ar", b, 2)] + HK, writes=[("ps", bank)])
                        if half == 0:
                            P.op("act", lambda e, yb=yb, bank=bank: e.activation(out=Yb[yb][:, 0:512], in_=ps[bank], func=AF.Copy),
                                 reads=[("ps", bank)], writes=[("Yb", yb, 0)])
                        else:
                            P.op("dve", lambda e, yb=yb, bank=bank: e.tensor_copy(out=Yb[yb][:, 512:1024], in_=ps[bank]),
                                 reads=[("ps", bank)], writes=[("Yb", yb, 1)])
                    r0 = (e_ * NJ + j) * 128
                    P.dmaf("pool", "ys%d" % yb, [lambda e, yb=yb, blk=e_ * NJ + j: e.indirect_dma_start(
                        out=Ys[:, :], out_offset=IOA(ap=idxY[:, blk:blk + 1], axis=0), in_=Yb[yb], in_offset=None,
                        bounds_check=breg(e), oob_is_err=False)], reads=[("Yb", yb, 0), ("Yb", yb, 1)], writes=[("Ys", e_, j)])
                if e_ + 2 < NE:
                    load_part(e_ + 2, 2)
            P.barrier()

            off = base
            NGB = 4
            Yg = []
            for _ in range(2 * NGB):
                a_, off = carve(off, [128, D], F32)
                Yg.append(a_)
            yt0, off = carve(off, [128, D], F32)
            yt1, off = carve(off, [128, D], F32)
            ytt = [yt0, yt1]
            for t in range(NT):
                b = t % 2
                gb = t % NGB
                g0, g1 = Yg[2 * gb], Yg[2 * gb + 1]
                for k, gk in enumerate((g0, g1)):
                    P.dmaf("pool", "ga%d%d" % (gb, k), [lambda e, gk=gk, t=t, k=k: e.indirect_dma_start(
                        out=gk, out_offset=None, in_=Ys[:, :], in_offset=IOA(ap=destg[k][:, t:t + 1], axis=0),
                        bounds_check=breg2(e), oob_is_err=False)], reads=["Ys"], writes=[("Yg", gb, k)])
                P.op("act", lambda e, b=b, g0=g0, t=t: e.activation(out=ytt[b], in_=g0, func=AF.Copy, scale=pw1[:, t:t + 1]),
                     reads=[("Yg", gb, 0)], writes=[("yt", b)])
                P.op("dve", lambda e, b=b, g1=g1, t=t: e.scalar_tensor_tensor(out=ytt[b], in0=g1, scalar=pw2[:, t:t + 1], in1=ytt[b],
                                                                              op0=ALU.mult, op1=ALU.add),
                     reads=[("Yg", gb, 1), ("yt", b)], writes=[("yt", b)])
                P.mm([lambda e, c=c, b=b: e.transpose(ps[2 * b + c // 4][:, (c % 4) * 128:(c % 4 + 1) * 128], ytt[b][:, c * 128:(c + 1) * 128], ident)
                      for c in range(NCH)], reads=[("yt", b), "consts"], writes=[("ps", 2 * b), ("ps", 2 * b + 1)])
                P.op("dve", lambda e, t=t, b=b: e.tensor_tensor(
                    out=h[:, :, t * 128:(t + 1) * 128], in0=h[:, :, t * 128:(t + 1) * 128],
                    in1=psA[:, b * 1024:(b + 1) * 1024].rearrange("p (c n) -> p c n", n=128), op=ALU.add),
                    reads=[("ps", 2 * b), ("ps", 2 * b + 1)] + [("h", c, t // 4) for c in range(NCH)],
                    writes=[("h", c, t // 4) for c in range(NCH)])
            P.barrier()

        def ple(l, pre=False):
            off = 0
            wpg, off = carve(off, [128, NCH, D], BF16)
            wpl, off = carve(off, [128, 2, D], BF16)
            pTs, off = carve(off, [128, 2, S], BF16)
            sg0, off = carve(off, [128, TW], F32)
            sg1, off = carve(off, [128, TW], F32)
            sg = [sg0, sg1]
            if pre:
                P.dma("pool", "wP", [(wpg, w_pg[l].rearrange("(kc p) d -> p kc d", p=128)),
                                     (wpl, w_ple[l].rearrange("(kc p) d -> p kc d", p=128)),
                                     (pTs, pT[l].rearrange("(kc p) t -> p kc t", p=128))], writes=["wple"])
                return
            i = 0
            for dc in range(NCH):
                for t in range(TG):
                    q = i % 2
                    ga = (i % 4)
                    pl = 4 + (i % 4)
                    i += 1
                    P.mm([MM(ps[ga], wpg[:, kc, dc * 128:(dc + 1) * 128], xn[:, kc, tsl(t)], kc == 0, kc == NCH - 1)
                          for kc in range(NCH)], reads=["wple"] + XNK, writes=[("ps", ga)])
                    P.mm([MM(ps[pl], wpl[:, kc, dc * 128:(dc + 1) * 128], pTs[:, kc, tsl(t)], kc == 0, kc == 1)
                          for kc in range(2)], reads=["wple"], writes=[("ps", pl)])
                    P.op("act", lambda e, dc=dc, ga=ga, q=q: e.activation(out=sg[q], in_=ps[ga], func=AF.Sigmoid,
                                                                          bias=vcol(V_BPLE + l, dc)),
                         reads=[("ps", ga), "consts"], writes=[("sg", q)])
                    P.op("dve", lambda e, q=q, pl=pl: e.tensor_tensor(out=sg[q], in0=sg[q], in1=ps[pl], op=ALU.mult),
                         reads=[("sg", q), ("ps", pl)], writes=[("sg", q)])
                    P.op("dve", lambda e, dc=dc, t=t, q=q: e.tensor_tensor(out=h[:, dc, tsl(t)], in0=h[:, dc, tsl(t)], in1=sg[q], op=ALU.add),
                         reads=[("sg", q), ("h", dc, t)], writes=[("h", dc, t)])
            P.barrier()

        def forward():
            for l in range(DEPTH):
                if l == 0:
                    mixer_a(pre=True)
                else:
                    mixer_b(pre=True)
                rmsnorm(3 * l + 0)
                if l == 0:
                    mixer_a()
                else:
                    mixer_b()
                if stop_after == ("mix", l):
                    return dump_h()
                router_prep(l)
                rmsnorm(3 * l + 1, tok=True, hook=router_mm)
                moe_sorted(l)
                if stop_after == ("moe", l):
                    return dump_h()
                ple(l, pre=True)
                rmsnorm(3 * l + 2)
                ple(l)
                if stop_after == ("ple", l):
                    return dump_h()
            rmsnorm(6, mode="out")
            P.finish()

        forward()
        P.replay()
    return nc


_NC_CACHE = {}


def _pack_cols(v):
    return np.ascontiguousarray(np.asarray(v, np.float32).reshape(8, 128).T)


def _consts():
    ident = np.eye(128, dtype=np.float32)
    tri = np.triu(np.ones((128, 128), np.float32), k=1)
    ebase = np.tile((np.arange(NE, dtype=np.float32) * CAP)[None, :], (128, 1))
    return np.ascontiguousarray(np.concatenate([ident, tri, ebase], axis=1))


def _pmajor(w):
    L_, E_, K_, N_ = w.shape
    return np.ascontiguousarray(w.reshape(L_, E_, K_ // 128, 128, N_).transpose(0, 1, 3, 2, 4)).reshape(L_, E_, 128, (K_ // 128) * N_)


def prepare_shared(inp):
    g = lambda k: np.asarray(inp[k], np.float32)
    blocks = []
    nm, nf, npl = g("norm_mix"), g("norm_ffn"), g("norm_ple")
    for l in range(DEPTH):
        blocks += [nm[l], nf[l], npl[l]]
    blocks.append(g("norm_final"))
    ca = g("conv_a")[0]
    blocks += [ca[0], ca[1], ca[2]]
    cb = g("conv_b")[0]
    blocks += [cb[0], cb[1], cb[2], cb[3]]
    blocks.append(g("conv_bias_b")[0])
    blocks += [g("b_rgate_b")[0][0], g("b_rgate_b")[0][1]]
    blocks += [g("b_igate_b")[0][0], g("b_igate_b")[0][1]]
    blocks += [g("lam_b")[0][0], g("lam_b")[0][1]]
    blocks += [g("b_ple_gate")[0], g("b_ple_gate")[1]]
    assert len(blocks) == NVB
    vecs = np.ascontiguousarray(np.concatenate([_pack_cols(b) for b in blocks], axis=1))
    wrc = np.concatenate([g("w_router_group"), g("w_router_expert")], axis=-1)
    wr = np.ascontiguousarray(wrc.reshape(DEPTH, 8, 128, 36).transpose(2, 0, 1, 3).reshape(128, -1))
    br = np.ascontiguousarray(np.concatenate([g("b_router_group"), g("b_router_expert")], axis=-1).reshape(-1))
    wg = np.stack([g("w_rgate_b")[0], g("w_igate_b")[0]], axis=0)
    wg = wg.reshape(2, 2, 4, 2, 128, 256).transpose(4, 0, 1, 2, 3, 5)
    wg = np.ascontiguousarray(wg.reshape(128, -1))
    shared = {
        "vecs": vecs, "wr": wr, "br": br, "cst": _consts(),
        "w_in_a": np.ascontiguousarray(g("w_in_a")[0]), "w_out_a": np.ascontiguousarray(g("w_out_a")[0]),
        "w_in_b": np.ascontiguousarray(g("w_in_b")[0]), "wgates": wg,
        "w_out_b": np.ascontiguousarray(g("w_out_b")[0]),
        "w_exp_gate": _pmajor(g("w_exp_gate")), "w_exp_up": _pmajor(g("w_exp_up")), "w_exp_down": _pmajor(g("w_exp_down")),
        "w_ple": g("w_ple"), "w_ple_gate": g("w_ple_gate"),
    }
    return shared


def make_in_maps(inp, cores):
    shared = prepare_shared(inp)
    x = np.asarray(inp["x"], np.float32)
    p = np.asarray(inp["p"], np.float32)
    maps = []
    for b in cores:
        m = dict(shared)
        m["xT"] = np.ascontiguousarray(x[b].T)
        m["pT"] = np.ascontiguousarray(p[:, b].transpose(0, 2, 1))
        maps.append(m)
    return maps


def kernel(**inputs):
    key = "full"
    if key not in _NC_CACHE:
        _NC_CACHE[key] = build_program()
    nc = _NC_CACHE[key]
    cores = list(range(8))
    in_maps = make_in_maps(inputs, cores)
    res = run_bass_kernel_spmd(nc, in_maps, core_ids=cores)
    out = np.stack([np.ascontiguousarray(r["yT"].T) for r in res.results], axis=0)
    return out.astype(np.float32)
```
